# Optimizing a Trainium2 kernel written in Bass

```python
import math
import jax, jax.numpy as jnp
from jax import lax
import numpy as np

D_MODEL = 2048
BATCH = 4
SEQ = 8192
DEPTH = 4

RET_HEADS = 8
RET_DK = D_MODEL // RET_HEADS
RET_DV = D_MODEL // RET_HEADS
RET_W = RET_HEADS * RET_DV
RET_CHUNK = 128
SGU_GROUPS = 8
SGU_W = D_MODEL
SGU_GC = SGU_W // SGU_GROUPS
SGU_CHUNK = 128
D_FF = 5632
ROPE_BASE = 10000.0
EPS = 1e-6
N_BRANCH = 2
IN_SPLITS = (RET_HEADS * RET_DK, RET_HEADS * RET_DK, RET_W, RET_W, SGU_W, SGU_W, D_MODEL, D_MODEL)
IN_COLS = sum(IN_SPLITS)

kernel_name = "hybrid_retention_sgu_macaron"


def _rmsnorm(x, g):
    xf = x.astype(jnp.float32)
    y = xf * lax.rsqrt(jnp.mean(xf * xf, axis=-1, keepdims=True) + EPS)
    return (y * g.astype(jnp.float32)).astype(x.dtype)


def _layernorm(x, g, axis_size_last=True):
    xf = x.astype(jnp.float32)
    mu = jnp.mean(xf, axis=-1, keepdims=True)
    var = jnp.mean(jnp.square(xf - mu), axis=-1, keepdims=True)
    y = (xf - mu) * lax.rsqrt(var + EPS)
    return y * g.astype(jnp.float32)


def _swiglu(x, w_gu, w_down):
    a, g = jnp.split(x @ w_gu, 2, axis=-1)
    return (jax.nn.silu(g) * a) @ w_down


def _rotary(x, positions):
    half = x.shape[-1] // 2
    inv = ROPE_BASE ** (-jnp.arange(half, dtype=jnp.float32) / half)
    ang = positions.astype(jnp.float32)[..., None] * inv
    cos = jnp.cos(ang)[:, :, None, :].astype(x.dtype)
    sin = jnp.sin(ang)[:, :, None, :].astype(x.dtype)
    x1, x2 = x[..., :half], x[..., half:]
    return jnp.concatenate([x1 * cos - x2 * sin, x1 * sin + x2 * cos], axis=-1)


def _retention(q, k, v):
    B, S, H, DK = q.shape
    DV = v.shape[-1]
    nc = S // RET_CHUNK
    dt = q.dtype
    log_gamma = jnp.log1p(-jnp.exp2(-5.0 - jnp.arange(H, dtype=jnp.float32)))
    idx = jnp.arange(RET_CHUNK, dtype=jnp.float32)
    diff = idx[:, None] - idx[None, :]
    causal = diff >= 0
    dmask = jnp.where(causal[None], jnp.exp(jnp.where(causal, diff, 0.0)[None] * log_gamma[:, None, None]), 0.0).astype(dt)
    xi = jnp.exp((idx[:, None] + 1.0) * log_gamma[None]).astype(dt)
    zeta = jnp.exp((RET_CHUNK - 1.0 - idx)[:, None] * log_gamma[None]).astype(dt)
    gamma_c = jnp.exp(RET_CHUNK * log_gamma).astype(dt)

    k = k * jnp.asarray(DK ** -0.5, dt)
    qc = q.reshape(B, nc, RET_CHUNK, H, DK)
    kc = k.reshape(B, nc, RET_CHUNK, H, DK)
    vc = v.reshape(B, nc, RET_CHUNK, H, DV)
    scores = jnp.einsum('bnihd,bnjhd->bnhij', qc, kc) * dmask
    intra = jnp.einsum('bnhij,bnjhe->bnihe', scores, vc)

    def step(R, inp):
        q_i, k_i, v_i = inp
        cross = jnp.einsum('bihd,bhde->bihe', q_i, R) * xi[None, :, :, None]
        R = R * gamma_c[None, :, None, None] + jnp.einsum('bjhd,bjhe->bhde', k_i * zeta[None, :, :, None], v_i)
        return R, cross

    R0 = jnp.zeros((B, H, DK, DV), dt)
    _, cross = lax.scan(step, R0, (jnp.moveaxis(qc, 1, 0), jnp.moveaxis(kc, 1, 0), jnp.moveaxis(vc, 1, 0)))
    cross = jnp.moveaxis(cross, 0, 1)
    return (intra + cross).reshape(B, S, H, DV)


def _spatial_gating(u, v, ln_g, w_s, b_s):
    B, S, _ = v.shape
    nc = S // SGU_CHUNK
    u = jax.nn.gelu(u, approximate=False)
    v = _layernorm(jax.nn.gelu(v, approximate=False), ln_g).astype(u.dtype)
    vg = v.reshape(B, nc, SGU_CHUNK, SGU_GROUPS, SGU_GC)
    tril = jnp.tril(jnp.ones((SGU_CHUNK, SGU_CHUNK), dtype=bool))
    w_m = jnp.where(tril[None], w_s, jnp.zeros((), w_s.dtype))
    mixed = jnp.einsum('gij,bnjgc->bnigc', w_m, vg) + b_s.T[None, None, :, :, None]
    return u * mixed.reshape(B, S, SGU_W)


def _layer(x, positions, ffn1_norm, ffn1_w_gu, ffn1_w_down, mix_norm, w_in, b_gate, ret_gn,
           sgu_ln, sgu_w, sgu_b, w_branch_ret, w_branch_sgu, w_out, ffn2_norm, ffn2_w_gu, ffn2_w_down):
    B, S, D = x.shape
    x = x + 0.5 * _swiglu(_rmsnorm(x, ffn1_norm), ffn1_w_gu, ffn1_w_down)
    h = _rmsnorm(x, mix_norm)
    proj = h @ w_in
    offs = list(np.cumsum(IN_SPLITS)[:-1])
    q, k, v, g_ret, u_s, v_s, gate_ret, gate_sgu = jnp.split(proj, offs, axis=-1)
    q = _rotary(q.reshape(B, S, RET_HEADS, RET_DK), positions)
    k = _rotary(k.reshape(B, S, RET_HEADS, RET_DK), positions)
    v = v.reshape(B, S, RET_HEADS, RET_DV)
    y_ret = _retention(q, k, v)
    y_ret = _layernorm(y_ret, ret_gn.reshape(RET_HEADS, RET_DV)).astype(x.dtype).reshape(B, S, RET_W)
    y_ret = jax.nn.silu(g_ret) * y_ret
    y_sgu = _spatial_gating(u_s, v_s, sgu_ln, sgu_w, sgu_b)
    gates = jax.nn.sigmoid(jnp.concatenate([gate_ret, gate_sgu], axis=-1) + b_gate)
    ga, gb = jnp.split(gates, 2, axis=-1)
    merged = ga * (y_ret @ w_branch_ret) + gb * (y_sgu @ w_branch_sgu)
    x = x + merged @ w_out
    x = x + 0.5 * _swiglu(_rmsnorm(x, ffn2_norm), ffn2_w_gu, ffn2_w_down)
    return x


def setup_inputs(seed: int = 0) -> dict:
    key = jax.random.key(seed)
    ks = jax.random.split(key, 20)
    L, D = DEPTH, D_MODEL

    def nrm(k, shape, scale):
        return jax.random.normal(k, shape, jnp.float32) * scale

    def gain(k, shape):
        return 1.0 + 0.02 * jax.random.normal(k, shape, jnp.float32)

    x = jax.random.normal(ks[0], (BATCH, SEQ, D), jnp.float32)
    positions = jnp.tile(jnp.arange(SEQ, dtype=jnp.int32)[None, :], (BATCH, 1))
    return {
        "x": x,
        "positions": positions,
        "ffn1_norm": gain(ks[1], (L, D)),
        "ffn1_w_gu": nrm(ks[2], (L, D, 2 * D_FF), D ** -0.5),
        "ffn1_w_down": nrm(ks[3], (L, D_FF, D), D_FF ** -0.5),
        "mix_norm": gain(ks[4], (L, D)),
        "w_in": nrm(ks[5], (L, D, IN_COLS), D ** -0.5),
        "b_gate": nrm(ks[6], (L, N_BRANCH * D), 0.02),
        "ret_gn": gain(ks[7], (L, RET_W)),
        "sgu_ln": gain(ks[8], (L, SGU_W)),
        "sgu_w": nrm(ks[9], (L, SGU_GROUPS, SGU_CHUNK, SGU_CHUNK), SGU_CHUNK ** -0.5),
        "sgu_b": gain(ks[10], (L, SGU_GROUPS, SGU_CHUNK)),
        "w_branch_ret": nrm(ks[11], (L, RET_W, D), RET_W ** -0.5),
        "w_branch_sgu": nrm(ks[12], (L, SGU_W, D), SGU_W ** -0.5),
        "w_out": nrm(ks[13], (L, D, D), D ** -0.5),
        "ffn2_norm": gain(ks[14], (L, D)),
        "ffn2_w_gu": nrm(ks[15], (L, D, 2 * D_FF), D ** -0.5),
        "ffn2_w_down": nrm(ks[16], (L, D_FF, D), D_FF ** -0.5),
        "final_norm": gain(ks[17], (D,)),
    }


def reference(x, positions, ffn1_norm, ffn1_w_gu, ffn1_w_down, mix_norm, w_in, b_gate, ret_gn,
              sgu_ln, sgu_w, sgu_b, w_branch_ret, w_branch_sgu, w_out, ffn2_norm, ffn2_w_gu,
              ffn2_w_down, final_norm):
    for l in range(DEPTH):
        x = _layer(x, positions, ffn1_norm[l], ffn1_w_gu[l], ffn1_w_down[l], mix_norm[l], w_in[l],
                   b_gate[l], ret_gn[l], sgu_ln[l], sgu_w[l], sgu_b[l], w_branch_ret[l],
                   w_branch_sgu[l], w_out[l], ffn2_norm[l], ffn2_w_gu[l], ffn2_w_down[l])
    return _rmsnorm(x, final_norm)
```

```python
import math
from contextlib import ExitStack

import numpy as np
import concourse.bass as bass
import concourse.mybir as mybir
from concourse.bass_utils import run_bass_kernel_spmd

F32 = mybir.dt.float32
BF16 = mybir.dt.bfloat16
I32 = mybir.dt.int32
AF = mybir.ActivationFunctionType
ALU = mybir.AluOpType
AX = mybir.AxisListType

D = 2048
DFF = 5632
NH = 8
T = 512
NCH = T // 128
EPS = 1e-6
NBL = 240
BLK = 4096
NS = 5
DEBUG = False
VPL = 112
TWO_PI = 2.0 * math.pi
CW1 = 6.28125
CW2 = TWO_PI - CW1

O_GU1, O_DN1, O_WIN, O_BRR, O_BRS, O_OUT, O_GU2, O_DN2 = 0, 44, 76, 140, 148, 156, 164, 208


class Buf:
    __slots__ = ("name", "w", "r")

    def __init__(self, name):
        self.name = name
        self.w = None
        self.r = {}


class Sched:
    def __init__(self, nc, stack):
        self.nc = nc
        self.eng = {"pe": nc.tensor, "act": nc.scalar, "dve": nc.vector,
                    "pool": nc.gpsimd, "sp": nc.sync}
        self.sems = {}
        self.cnt = {}
        self.waited = {e: {} for e in self.eng}
        self.stack = stack
        self.dry = False
        for e in self.eng:
            self.sems["e_" + e] = stack.enter_context(nc.semaphore("sem_" + e))
            self.cnt["e_" + e] = 0
        self.n_inst = 0

    def new_sem(self, name):
        key = "d_" + name
        self.sems[key] = self.stack.enter_context(self.nc.semaphore("sem_" + name))
        self.cnt[key] = 0
        return key

    def _deps(self, reads, writes):
        deps = {}
        for b in reads:
            if b.w is not None:
                k, v = b.w
                if deps.get(k, 0) < v:
                    deps[k] = v
        for b in writes:
            if b.w is not None:
                k, v = b.w
                if deps.get(k, 0) < v:
                    deps[k] = v
            for k, v in b.r.items():
                if deps.get(k, 0) < v:
                    deps[k] = v
        return deps

    def _wait(self, e, deps):
        own = "e_" + e
        for k, v in deps.items():
            if k == own and (e == "pe" or v > self.cnt[own]):
                continue
            if self.waited[e].get(k, 0) >= v:
                continue
            self.eng[e].wait_ge(self.sems[k], v)
            self.waited[e][k] = v

    def _mark(self, tok, reads, writes):
        for b in reads:
            if b.r.get(tok[0], 0) < tok[1]:
                b.r[tok[0]] = tok[1]
        for b in writes:
            b.w = tok
            b.r = {}

    def op(self, e, fn, reads=(), writes=(), inc=True):
        if self.dry:
            return None
        self._wait(e, self._deps(reads, writes))
        ins = fn(self.eng[e])
        self.n_inst += 1
        key = "e_" + e
        if inc:
            self.cnt[key] += 1
            ins.then_inc(self.sems[key], 1)
            tok = (key, self.cnt[key])
        else:
            tok = (key, self.cnt[key] + 1)
        self._mark(tok, reads, writes)
        return ins

    def dma(self, q, out, in_, semkey, reads=(), writes=()):
        if self.dry:
            return None
        self._wait(q, self._deps(reads, writes))
        ins = self.eng[q].dma_start(out=out, in_=in_)
        self.n_inst += 1
        self.cnt[semkey] += 16
        ins.then_inc(self.sems[semkey], 16)
        self._mark((semkey, self.cnt[semkey]), reads, writes)
        return ins

    def wait_all(self, e, bufs):
        if self.dry:
            return
        self._wait(e, self._deps(bufs, bufs))


def gammas():
    h = np.arange(NH, dtype=np.float64)
    return np.log1p(-np.exp2(-5.0 - h))


def build_nc(L, NTT):
    NT = NTT * T
    NB = L * NBL
    lg = gammas()
    gamma_c = [float(np.exp(128.0 * lg[h])) for h in range(NH)]

    nc = bass.Bass("TRN2", target_bir_lowering=False)
    x_in = nc.dram_tensor("x", [NTT * 128, 16 * T], F32, kind="ExternalInput").ap()
    pos_in = nc.dram_tensor("pos", [1, NT], I32, kind="ExternalInput").ap()
    wsrc = nc.dram_tensor("wsrc", [NB * 128, BLK], F32, kind="ExternalInput").ap()
    vecs_in = nc.dram_tensor("vecs", [128, L * VPL + 16], F32, kind="ExternalInput").ap()
    sguw_in = nc.dram_tensor("sguw", [128, L * NH * 128], F32, kind="ExternalInput").ap()
    sgub_in = nc.dram_tensor("sgub", [1, L * NH * 128], F32, kind="ExternalInput").ap()
    cst_in = nc.dram_tensor("cst", [128, 2400 + NTT * 32], F32, kind="ExternalInput").ap()
    out_d = nc.dram_tensor("out", [NTT * 128, 16 * T], F32, kind="ExternalOutput").ap()
    if DEBUG:
        dbgA = nc.dram_tensor("dbgA", [NTT * 128, 16 * T], F32, kind="ExternalOutput").ap()
        dbgM = nc.dram_tensor("dbgM", [NTT * 128, 16 * T], F32, kind="ExternalOutput").ap()
    xs_t = nc.dram_tensor("xs", [NTT * 128, 16 * T], F32)
    wb_t = [nc.dram_tensor("wb%d" % l, [NBL * 128, BLK], BF16) for l in range(L)]
    cs_t = nc.dram_tensor("cs", [2 * 128, NT], F32)
    cci_t = [nc.dram_tensor("cci%d" % l, [128, 4096], F32) for l in range(L)]
    cco_t = [nc.dram_tensor("cco%d" % l, [256, 4096], F32) for l in range(L)]
    xs = xs_t.ap()
    wbl = [w.ap() for w in wb_t]

    def wb_rows(b):
        return wbl[b // NBL][(b % NBL) * 128:(b % NBL + 1) * 128, :]
    cs = cs_t.ap()

    with ExitStack() as st:
        S = Sched(nc, st)

        def sb(name, shape, dt):
            return st.enter_context(nc.sbuf_tensor(name, shape, dt))

        X = sb("X", [128, 16 * T], F32)
        bX = Buf("X")
        HN = sb("HN", [128, 16 * T], BF16)
        bHN = Buf("HN")
        R = sb("R", [128, 32768], BF16)
        bG = [Buf("g%d" % i) for i in range(8)]
        SL = [sb("SL%d" % i, [128, BLK], BF16) for i in range(NS)]
        bSL = [Buf("SL%d" % i) for i in range(NS)]
        S32 = sb("S32", [128, NH * 512], F32)
        bS32 = [Buf("S32_%d" % h) for h in range(NH)]
        SB = sb("SB", [128, 4 * 512], BF16)
        bSB = [Buf("SB%d" % i) for i in range(4)]
        CS = sb("CS", [128, 2 * T], F32)
        bCS = Buf("CS")
        CST = sb("CST", [128, 2400 + NTT * 32], F32)
        bCST = Buf("CST")
        VEC = sb("VEC", [128, L * VPL + 16], F32)
        bVEC = Buf("VEC")
        WS = sb("WS", [128, NH * 128], BF16)
        bWS = Buf("WS")
        BS = sb("BS", [128, NH * 128], F32)
        bBS = Buf("BS")
        ONEB = sb("ONEB", [128, 128], BF16)
        ONEF = sb("ONEF", [128, 128], F32)
        IDB = sb("IDB", [128, 128], BF16)
        bONE = Buf("ONE")
        TMP = [sb("TMP%d" % i, [128, T], F32) for i in range(3)]
        bTMP = [Buf("TMP%d" % i) for i in range(3)]
        RSTD = sb("RSTD", [128, T], F32)
        bRSTD = Buf("RSTD")
        SQ = [sb("SQ%d" % i, [128, T], BF16) for i in range(2)]
        bSQ = [Buf("SQ%d" % i) for i in range(2)]
        PM = [sb("PM%d" % i, [128, T], BF16) for i in range(2)]
        bPM = [Buf("PM%d" % i) for i in range(2)]
        ST = sb("ST", [128, 64], F32)
        bST = Buf("ST")
        PS = [st.enter_context(nc.psum_tensor("PS%d" % i, [128, 512], F32)) for i in range(8)]
        bPS = [Buf("PS%d" % i) for i in range(8)]
        psi = [0]

        def nb():
            i = psi[0] % 8
            psi[0] += 1
            return PS[i], bPS[i]

        tmi = [0]

        def ntmp():
            i = tmi[0] % 3
            tmi[0] += 1
            return TMP[i], bTMP[i]

        C_MT, C_XI, C_MASK, C_INV, C_ZT, C_FLAG, C_EPS, C_WA = 0, 1024, 2048, 2176, 2184, 2192, 2193, 2400

        def rv_bf(g0, n):
            return R[:, g0 * 4096:g0 * 4096 + n]

        def rv_f32(g0, n):
            return R[:, g0 * 4096:g0 * 4096 + 2 * n].bitcast(F32)

        Hh = rv_bf(0, 44 * T)

        def bH(fc):
            return bG[(fc * T) // 4096]

        sem_w = [S.new_sem("w%d" % i) for i in range(NS)]
        sem_cst = S.new_sem("cst")
        sem_vec = S.new_sem("vec")
        sem_pos = S.new_sem("pos")
        sem_sguw = S.new_sem("sguw")
        sem_bs = S.new_sem("bs")
        sem_cci = S.new_sem("cci")
        sem_s32 = S.new_sem("s32")
        sem_x = S.new_sem("x")
        sem_csl = [S.new_sem("csl0"), S.new_sem("csl1")]
        sem_st = S.new_sem("st")
        sem_pc = [S.new_sem("pc0"), S.new_sem("pc1")]
        sem_pcs = [S.new_sem("pcs%d" % i) for i in range(NS)]
        sem_cs = S.new_sem("cs")
        sem_cc = [S.new_sem("cc%d" % l) for l in range(L)]
        sem_out = S.new_sem("out")
        bXS = [Buf("xs%d" % t) for t in range(NTT)]
        bWB = [Buf("wb%d" % b) for b in range(NB)]
        bCSD = Buf("csd")

        class WStream:
            def __init__(self):
                self.sched = []
                self.i = 0
                self.loaded = 0
                self.released = []
                self.held = []

            def reset(self):
                self.i = 0
                self.loaded = 0
                self.released = [False] * len(self.sched)
                self.held = []

            def _fill(self):
                while (self.loaded < len(self.sched) and self.loaded < self.i + NS
                       and (self.loaded < NS or self.released[self.loaded - NS])):
                    j = self.loaded
                    s = j % NS
                    b = self.sched[j]
                    S.dma("sp", SL[s][:], wb_rows(b), sem_w[s],
                          reads=[bWB[b]], writes=[bSL[s]])
                    self.loaded += 1

            def pop(self, blk):
                if S.dry:
                    self.sched.append(blk)
                    return SL[0], bSL[0]
                i = self.i
                assert self.sched[i] == blk, (i, self.sched[i], blk)
                self._fill()
                assert self.loaded > i, "weight slot ring exhausted (too many blocks held)"
                self.held.append(i)
                self.i += 1
                return SL[i % NS], bSL[i % NS]

            def release(self):
                if S.dry:
                    return
                for i in self.held:
                    self.released[i] = True
                self.held = []
                self._fill()

        ws = WStream()

        def mm(out_ap, bbank, pairs, reads):
            n = len(pairs)
            for i, (l, r) in enumerate(pairs):
                S.op("pe", lambda e: e.matmul(out_ap, l, r, start=(i == 0), stop=(i == n - 1)),
                     reads=reads, writes=[bbank], inc=(i == n - 1))

        def rstd_from(ps_ap, bps, scale):
            S.op("dve", lambda e: e.tensor_scalar(RSTD[:], ps_ap, scale, EPS, ALU.mult, ALU.add),
                 reads=[bps], writes=[bRSTD])
            S.op("act", lambda e: e.activation(RSTD[:], RSTD[:], AF.Sqrt), reads=[bRSTD], writes=[bRSTD])
            S.op("dve", lambda e: e.reciprocal(RSTD[:], RSTD[:]), reads=[bRSTD], writes=[bRSTD])

        def rmsnorm(gcol, out_f32=False):
            pst, bps = nb()
            for c in range(16):
                sq, bsq = SQ[c % 2], bSQ[c % 2]
                S.op("act", lambda e: e.activation(sq[:], X[:, c * T:(c + 1) * T], AF.Square),
                     reads=[bX], writes=[bsq])
                S.op("pe", lambda e: e.matmul(pst[:], ONEB[:], sq[:], start=(c == 0), stop=(c == 15)),
                     reads=[bsq, bONE], writes=[bps], inc=True)
            rstd_from(pst[:], bps, 1.0 / D)
            for c in range(16):
                if out_f32:
                    S.op("dve", lambda e: e.scalar_tensor_tensor(
                        X[:, c * T:(c + 1) * T], X[:, c * T:(c + 1) * T], VEC[:, gcol + c:gcol + c + 1],
                        RSTD[:], ALU.mult, ALU.mult), reads=[bX, bRSTD, bVEC], writes=[bX])
                else:
                    S.op("dve", lambda e: e.scalar_tensor_tensor(
                        HN[:, c * T:(c + 1) * T], X[:, c * T:(c + 1) * T], VEC[:, gcol + c:gcol + c + 1],
                        RSTD[:], ALU.mult, ALU.mult), reads=[bX, bRSTD, bVEC], writes=[bHN])

        def ffn(base, o_gu, o_dn):
            for fb in range(22):
                Wa, bWa = ws.pop(base + o_gu + fb)
                Wg, bWg = ws.pop(base + o_gu + 22 + fb)
                for fc in range(2):
                    pa, bpa = nb()
                    pg, bpg = nb()
                    mm(pa[:], bpa, [(Wa[:, kc * 256 + fc * 128: kc * 256 + fc * 128 + 128], HN[:, kc * T:(kc + 1) * T])
                                    for kc in range(16)], [bWa, bHN])
                    mm(pg[:], bpg, [(Wg[:, kc * 256 + fc * 128: kc * 256 + fc * 128 + 128], HN[:, kc * T:(kc + 1) * T])
                                    for kc in range(16)], [bWg, bHN])
                    tm, btm = ntmp()
                    S.op("act", lambda e: e.activation(tm[:], pg[:], AF.Silu), reads=[bpg], writes=[btm])
                    f = fb * 2 + fc
                    S.op("dve", lambda e: e.tensor_tensor(Hh[:, f * T:(f + 1) * T], pa[:], tm[:], ALU.mult),
                         reads=[bpa, btm], writes=[bH(f)])
                ws.release()
            for dc in range(16):
                po, bpo = nb()
                for half in range(2):
                    Wd, bWd = ws.pop(base + o_dn + dc * 2 + half)
                    for fl in range(22):
                        f = half * 22 + fl
                        S.op("pe", lambda e: e.matmul(po[:], Wd[:, fl * 128:(fl + 1) * 128], Hh[:, f * T:(f + 1) * T],
                                                      start=(f == 0), stop=(f == 43)),
                             reads=[bWd, bH(f)], writes=[bpo], inc=(fl == 21))
                    ws.release()
                S.op("dve", lambda e: e.scalar_tensor_tensor(
                    X[:, dc * T:(dc + 1) * T], po[:], 0.5, X[:, dc * T:(dc + 1) * T], ALU.mult, ALU.add),
                    reads=[bpo, bX], writes=[bX])

        def rotary(pa, bpa, pb, bpb, dst1, dst2, bdst):
            cos = CS[:, 0:T]
            sin = CS[:, T:2 * T]
            t0, bt0 = TMP[0], bTMP[0]
            t1, bt1 = TMP[1], bTMP[1]
            S.op("dve", lambda e: e.tensor_tensor(t0[:], pa[:], cos, ALU.mult), reads=[bpa, bCS], writes=[bt0])
            S.op("dve", lambda e: e.tensor_tensor(t1[:], pb[:], sin, ALU.mult), reads=[bpb, bCS], writes=[bt1])
            S.op("dve", lambda e: e.tensor_tensor(dst1, t0[:], t1[:], ALU.subtract), reads=[bt0, bt1], writes=[bdst])
            S.op("dve", lambda e: e.tensor_tensor(t0[:], pa[:], sin, ALU.mult), reads=[bpa, bCS], writes=[bt0])
            S.op("dve", lambda e: e.tensor_tensor(t1[:], pb[:], cos, ALU.mult), reads=[bpb, bCS], writes=[bt1])
            S.op("dve", lambda e: e.tensor_tensor(dst2, t0[:], t1[:], ALU.add), reads=[bt0, bt1], writes=[bdst])

        def proj_fm(W, bW, fc):
            p, bp = nb()
            mm(p[:], bp, [(W[:, kc * 256 + fc * 128: kc * 256 + fc * 128 + 128], HN[:, kc * T:(kc + 1) * T])
                          for kc in range(16)], [bW, bHN])
            return p, bp

        def proj_head_rot(blk, dst, bdst, hl):
            W, bW = ws.pop(blk)
            pa, bpa = proj_fm(W, bW, 0)
            pb, bpb = proj_fm(W, bW, 1)
            ws.release()
            rotary(pa, bpa, pb, bpb, dst[:, (2 * hl) * T:(2 * hl + 1) * T], dst[:, (2 * hl + 1) * T:(2 * hl + 2) * T], bdst)

        def proj_v_tm(blk, dst, bdst, col0, width_total, func=None, accum_col=None):
            W, bW = ws.pop(blk)
            for ch in range(NCH):
                p, bp = nb()
                mm(p[:, 0:256], bp, [(HN[:, kc * T + ch * 128: kc * T + ch * 128 + 128], W[:, kc * 256:(kc + 1) * 256])
                                     for kc in range(16)], [bW, bHN])
                o = dst[:, ch * width_total + col0: ch * width_total + col0 + 256]
                if func is None:
                    S.op("act", lambda e: e.activation(o, p[:, 0:256], AF.Copy), reads=[bp], writes=[bdst[ch]])
                else:
                    S.op("act", lambda e: e.activation(o, p[:, 0:256], func, accum_out=ST[:, accum_col(ch):accum_col(ch) + 1]),
                         reads=[bp], writes=[bdst[ch], bST])
            ws.release()

        def program():
            ws.reset()
            S.dma("sp", CST[:], cst_in, sem_cst, writes=[bCST])
            S.dma("sp", VEC[:], vecs_in, sem_vec, writes=[bVEC])
            S.op("dve", lambda e: e.memset(ONEF[:], 1.0), writes=[bONE])
            S.op("dve", lambda e: e.tensor_copy(ONEB[:], ONEF[:]), reads=[bONE], writes=[bONE])
            S.op("dve", lambda e: e.tensor_copy(IDB[:], CST[:, 2208:2336]), reads=[bCST, bONE], writes=[bONE])
            for t in range(NTT):
                PI_, bPI = TMP[2], bTMP[2]
                pi_i = PI_[:].bitcast(I32)
                S.dma("sp", pi_i, pos_in[:, t * T:(t + 1) * T].partition_broadcast(128), sem_pos, writes=[bPI])
                ang, bang = TMP[0], bTMP[0]
                S.op("dve", lambda e: e.tensor_copy(ang[:], pi_i), reads=[bPI], writes=[bang])
                S.op("dve", lambda e: e.tensor_scalar(ang[:], ang[:], CST[:, C_INV:C_INV + 1], None, ALU.mult),
                     reads=[bang, bCST], writes=[bang])
                for which in range(2):
                    a2, ba2 = TMP[1], bTMP[1]
                    shift = (math.pi / 2.0) if which == 0 else 0.0
                    S.op("dve", lambda e: e.tensor_scalar(a2[:], ang[:], shift, 1.0 / TWO_PI, ALU.add, ALU.mult),
                         reads=[bang], writes=[ba2])
                    S.op("dve", lambda e: e.tensor_copy(pi_i, a2[:]), reads=[ba2], writes=[bPI])
                    S.op("dve", lambda e: e.tensor_copy(a2[:], pi_i), reads=[bPI], writes=[ba2])
                    r, br = RSTD, bRSTD
                    S.op("dve", lambda e: e.scalar_tensor_tensor(r[:], a2[:], -CW1, ang[:], ALU.mult, ALU.add),
                         reads=[ba2, bang], writes=[br])
                    S.op("dve", lambda e: e.scalar_tensor_tensor(r[:], a2[:], -CW2, r[:], ALU.mult, ALU.add),
                         reads=[ba2, br], writes=[br])
                    S.op("dve", lambda e: e.tensor_scalar(r[:], r[:], shift, None, ALU.add), reads=[br], writes=[br])
                    S.op("dve", lambda e: e.tensor_scalar(r[:], r[:], math.pi, -math.pi, ALU.min, ALU.max),
                         reads=[br], writes=[br])
                    S.op("act", lambda e: e.activation(CS[:, which * T:(which + 1) * T], r[:], AF.Sin),
                         reads=[br], writes=[bCS])
                for which in range(2):
                    S.dma("sp", cs[which * 128:(which + 1) * 128, t * T:(t + 1) * T], CS[:, which * T:(which + 1) * T],
                          sem_cs, reads=[bCS], writes=[bCSD])
            STG = [X[:, 0:BLK], X[:, BLK:2 * BLK]]
            bSTG = [Buf("stg0"), Buf("stg1")]
            S.wait_all("sp", [bX])
            for b in range(NB):
                sgi = b % 2
                sl = b % NS
                S.dma("sp", STG[sgi], wsrc[b * 128:(b + 1) * 128, :], sem_pc[sgi], writes=[bSTG[sgi], bX])
                if b % 2 == 0:
                    S.op("act", lambda e: e.activation(SL[sl][:], STG[sgi], AF.Copy), reads=[bSTG[sgi], bX], writes=[bSL[sl]])
                else:
                    S.op("dve", lambda e: e.tensor_copy(SL[sl][:], STG[sgi]), reads=[bSTG[sgi], bX], writes=[bSL[sl]])
                S.dma("sp", wb_rows(b), SL[sl][:], sem_pcs[sl], reads=[bSL[sl]], writes=[bWB[b]])

            for l in range(L):
                base = l * NBL
                vb = l * VPL
                tmw, btmw = TMP[2], bTMP[2]
                for half in range(2):
                    S.dma("sp", tmw[:], sguw_in[:, (l * NH + half * 4) * 128:(l * NH + half * 4 + 4) * 128], sem_sguw, writes=[btmw])
                    S.op("dve", lambda e: e.tensor_tensor(
                        WS[:, half * 512:(half + 1) * 512].rearrange("p (g i) -> p g i", g=4),
                        tmw[:].rearrange("p (g i) -> p g i", g=4),
                        CST[:, C_MASK:C_MASK + 128].unsqueeze(1).to_broadcast([128, 4, 128]), ALU.mult),
                        reads=[btmw, bCST], writes=[bWS])
                S.dma("sp", BS[:], sgub_in[:, l * NH * 128:(l + 1) * NH * 128].partition_broadcast(128), sem_bs, writes=[bBS])
                for h in range(NH):
                    S.op("dve", lambda e: e.memset(S32[:, h * 512:(h + 1) * 512], 0.0), writes=[bS32[h]])

                KT = rv_bf(0, 16 * T)
                bKT = Buf("KTa")
                KW = rv_bf(2, 4 * 2048)
                VV = rv_bf(4, 4 * 2048)
                for t in range(NTT):
                    src = x_in if l == 0 else xs
                    S.dma("sp", X[:], src[t * 128:(t + 1) * 128, :], sem_x, reads=[bXS[t]], writes=[bX])
                    for which in range(2):
                        S.dma("sp", CS[:, which * T:(which + 1) * T], cs[which * 128:(which + 1) * 128, t * T:(t + 1) * T],
                              sem_csl[which], reads=[bCSD], writes=[bCS])
                    rmsnorm(vb + 0)
                    ffn(base, O_GU1, O_DN1)
                    S.dma("sp", xs[t * 128:(t + 1) * 128, :], X[:], sem_st, reads=[bX], writes=[bXS[t]])
                    if DEBUG and l == 0:
                        S.dma("sp", dbgA[t * 128:(t + 1) * 128, :], X[:], sem_out, reads=[bX])
                    rmsnorm(vb + 16)
                    bk = [bG[0], bG[1]]
                    for h in range(NH):
                        W, bW = ws.pop(base + O_WIN + 8 + h)
                        pa, bpa = proj_fm(W, bW, 0)
                        pb, bpb = proj_fm(W, bW, 1)
                        ws.release()
                        rotary(pa, bpa, pb, bpb, KT[:, (2 * h) * T:(2 * h + 1) * T], KT[:, (2 * h + 1) * T:(2 * h + 2) * T], bk[h // 4])
                    for h in range(NH):
                        proj_v_tm(base + O_WIN + 16 + h, VV, [bG[4], bG[4], bG[5], bG[5]], h * 256, 2048)
                    bKTall = Buf("x")

                    def tabA(ch, h0, hcnt, t=t):
                        n = t * NCH + ch
                        return CST[:, C_WA + n * 8 + h0: C_WA + n * 8 + h0 + hcnt].unsqueeze(2).to_broadcast([128, hcnt, 256])
                    for ch in range(NCH):
                        for half in range(2):
                            p, bp = nb()
                            pbv = p[:].bitcast(BF16)
                            for c8 in range(8):
                                c = half * 8 + c8
                                S.op("pe", lambda e: e.transpose(pbv[:, c8 * 128:(c8 + 1) * 128],
                                                                 KT[:, c * T + ch * 128: c * T + ch * 128 + 128], IDB[:]),
                                     reads=[bk[half], bONE], writes=[bp], inc=(c8 == 7))
                            o = KW[:, ch * 2048 + half * 1024: ch * 2048 + half * 1024 + 1024]
                            S.op("dve", lambda e: e.tensor_tensor(
                                o.rearrange("p (h d) -> p h d", h=4),
                                pbv[:, 0:1024].rearrange("p (h d) -> p h d", h=4),
                                tabA(ch, half * 4, 4), ALU.mult), reads=[bp, bCST], writes=[bG[2 + ch // 2]])
                    for h in range(NH):
                        pu, bpu = nb()
                        for dc in range(2):
                            mm(pu[:, dc * 256:(dc + 1) * 256], bpu,
                               [(KW[:, ch * 2048 + h * 256 + dc * 128: ch * 2048 + h * 256 + dc * 128 + 128],
                                 VV[:, ch * 2048 + h * 256: ch * 2048 + (h + 1) * 256]) for ch in range(NCH)],
                               [bG[2], bG[3], bG[4], bG[5]])
                        S.op("dve", lambda e: e.tensor_tensor(S32[:, h * 512:(h + 1) * 512], S32[:, h * 512:(h + 1) * 512],
                                                              pu[:], ALU.add), reads=[bpu, bS32[h]], writes=[bS32[h]])

                bcci = Buf("cci")
                bcco = Buf("cco")
                S.dma("pool", cci_t[l].ap(), S32[:], sem_cci, reads=bS32, writes=[bcci])
                S.wait_all("pool", [bcci])
                if not S.dry:
                    ins = nc.gpsimd.collective_compute("AllGather", ALU.bypass,
                                                       replica_groups=[[0, 1], [2, 3], [4, 5], [6, 7]],
                                                       ins=[cci_t[l].ap().opt()], outs=[cco_t[l].ap().opt()])
                    S.cnt[sem_cc[l]] += 1
                    ins.then_inc(S.sems[sem_cc[l]], 1)
                    bcco.w = (sem_cc[l], 1)
                S.dma("sp", S32[:], cco_t[l].ap()[0:128, :], sem_s32, reads=[bcco], writes=bS32)
                for h in range(NH):
                    S.op("dve", lambda e: e.tensor_scalar(S32[:, h * 512:(h + 1) * 512], S32[:, h * 512:(h + 1) * 512],
                                                          CST[:, C_FLAG:C_FLAG + 1], None, ALU.mult),
                         reads=[bS32[h], bCST], writes=[bS32[h]])

                QT = rv_bf(0, 8 * T)
                KT2 = rv_bf(1, 8 * T)
                KZ = rv_bf(2, 4 * 1024)
                V4 = rv_bf(3, 4 * 1024)
                YP = rv_f32(4, 8 * T)
                YR = rv_bf(6, 16 * T)
                VG = rv_f32(0, 4 * 2048)
                VLN = rv_bf(4, 4 * 2048)
                YS = rv_bf(0, 16 * T)
                MG = rv_bf(2, 16 * T)
                last = (l == L - 1)
                for t in range(NTT):
                    S.dma("sp", X[:], xs[t * 128:(t + 1) * 128, :], sem_x, reads=[bXS[t]], writes=[bX])
                    for which in range(2):
                        S.dma("sp", CS[:, which * T:(which + 1) * T], cs[which * 128:(which + 1) * 128, t * T:(t + 1) * T],
                              sem_csl[which], reads=[bCSD], writes=[bCS])
                    rmsnorm(vb + 16)
                    for hh in range(2):
                        for hl in range(4):
                            h = hh * 4 + hl
                            proj_head_rot(base + O_WIN + h, QT, bG[0], hl)
                        for hl in range(4):
                            h = hh * 4 + hl
                            proj_head_rot(base + O_WIN + 8 + h, KT2, bG[1], hl)
                        for hl in range(4):
                            h = hh * 4 + hl
                            proj_v_tm(base + O_WIN + 16 + h, V4, [bG[3]] * 4, hl * 256, 1024)
                        for ch in range(NCH):
                            p, bp = nb()
                            pbv = p[:].bitcast(BF16)
                            for c in range(8):
                                S.op("pe", lambda e: e.transpose(pbv[:, c * 128:(c + 1) * 128],
                                                                 KT2[:, c * T + ch * 128: c * T + ch * 128 + 128], IDB[:]),
                                     reads=[bG[1], bONE], writes=[bp], inc=(c == 7))
                            S.op("dve", lambda e: e.tensor_tensor(
                                KZ[:, ch * 1024:(ch + 1) * 1024].rearrange("p (h d) -> p h d", h=4),
                                pbv[:, 0:1024].rearrange("p (h d) -> p h d", h=4),
                                CST[:, C_ZT + hh * 4: C_ZT + hh * 4 + 4].unsqueeze(2).to_broadcast([128, 4, 256]), ALU.mult),
                                reads=[bp, bCST], writes=[bG[2]])
                        for hl in range(4):
                            h = hh * 4 + hl
                            U = []
                            for ch in range(NCH):
                                pu, bpu = nb()
                                for dc in range(2):
                                    mm(pu[:, dc * 256:(dc + 1) * 256], bpu,
                                       [(KZ[:, ch * 1024 + hl * 256 + dc * 128: ch * 1024 + hl * 256 + dc * 128 + 128],
                                         V4[:, ch * 1024 + hl * 256: ch * 1024 + (hl + 1) * 256])], [bG[2], bG[3]])
                                U.append((pu, bpu))
                            psc, bpsc = nb()
                            for ch in range(NCH):
                                mm(psc[:, ch * 128:(ch + 1) * 128], bpsc,
                                   [(KT2[:, (2 * hl + dc) * T + ch * 128: (2 * hl + dc) * T + ch * 128 + 128],
                                     QT[:, (2 * hl + dc) * T + ch * 128: (2 * hl + dc) * T + ch * 128 + 128]) for dc in range(2)],
                                   [bG[0], bG[1]])
                            pm, bpm = PM[h % 2], bPM[h % 2]
                            S.op("dve", lambda e: e.tensor_tensor(
                                pm[:].rearrange("p (c i) -> p c i", c=4), psc[:].rearrange("p (c i) -> p c i", c=4),
                                CST[:, C_MT + h * 128: C_MT + (h + 1) * 128].unsqueeze(1).to_broadcast([128, 4, 128]), ALU.mult),
                                reads=[bpsc, bCST], writes=[bpm])
                            for ch in range(NCH):
                                S.op("act", lambda e: e.activation(SB[:, ch * 512:(ch + 1) * 512], S32[:, h * 512:(h + 1) * 512], AF.Copy),
                                     reads=[bS32[h]], writes=[bSB[ch]])
                                pu, bpu = U[ch]
                                S.op("dve", lambda e: e.scalar_tensor_tensor(
                                    S32[:, h * 512:(h + 1) * 512], S32[:, h * 512:(h + 1) * 512], gamma_c[h], pu[:], ALU.mult, ALU.add),
                                    reads=[bS32[h], bpu], writes=[bS32[h]])
                            for ec in range(2):
                                py, bpy = nb()
                                for ch in range(NCH):
                                    prs = [(V4[:, ch * 1024 + hl * 256 + ec * 128: ch * 1024 + hl * 256 + ec * 128 + 128],
                                            pm[:, ch * 128:(ch + 1) * 128])]
                                    for dc in range(2):
                                        prs.append((SB[:, ch * 512 + dc * 256 + ec * 128: ch * 512 + dc * 256 + ec * 128 + 128],
                                                    QT[:, (2 * hl + dc) * T + ch * 128: (2 * hl + dc) * T + ch * 128 + 128]))
                                    mm(py[:, ch * 128:(ch + 1) * 128], bpy, prs, [bG[3], bpm, bSB[ch], bG[0]])
                                c = 2 * hl + ec
                                S.op("dve", lambda e: e.tensor_tensor(
                                    YP[:, c * T:(c + 1) * T].rearrange("p (c i) -> p c i", c=4), py[:].rearrange("p (c i) -> p c i", c=4),
                                    CST[:, C_XI + h * 128: C_XI + (h + 1) * 128].unsqueeze(1).to_broadcast([128, 4, 128]), ALU.mult),
                                    reads=[bpy, bCST], writes=[bG[4 + c // 4]])
                            p1, bp1 = nb()
                            p2, bp2 = nb()
                            sqs = []
                            for ec in range(2):
                                c = 2 * hl + ec
                                tq, btq = TMP[ec], bTMP[ec]
                                S.op("act", lambda e: e.activation(tq[:], YP[:, c * T:(c + 1) * T], AF.Square),
                                     reads=[bG[4 + c // 4]], writes=[btq])
                                sqs.append((tq, btq))
                            for ec in range(2):
                                c = 2 * hl + ec
                                S.op("pe", lambda e: e.matmul(p1[:], ONEF[:], YP[:, c * T:(c + 1) * T], start=(ec == 0), stop=(ec == 1)),
                                     reads=[bG[4 + c // 4], bONE], writes=[bp1], inc=(ec == 1))
                            for ec in range(2):
                                tq, btq = sqs[ec]
                                S.op("pe", lambda e: e.matmul(p2[:], ONEF[:], tq[:], start=(ec == 0), stop=(ec == 1)),
                                     reads=[btq, bONE], writes=[bp2], inc=(ec == 1))
                            mean, bmean = TMP[2], bTMP[2]
                            S.op("dve", lambda e: e.tensor_scalar(mean[:], p1[:], 1.0 / 256.0, None, ALU.mult), reads=[bp1], writes=[bmean])
                            m2, bm2 = TMP[0], bTMP[0]
                            S.op("act", lambda e: e.activation(m2[:], p1[:], AF.Square, scale=1.0 / 256.0), reads=[bp1], writes=[bm2])
                            S.op("dve", lambda e: e.scalar_tensor_tensor(RSTD[:], p2[:], 1.0 / 256.0, m2[:], ALU.mult, ALU.subtract),
                                 reads=[bp2, bm2], writes=[bRSTD])
                            S.op("dve", lambda e: e.tensor_scalar(RSTD[:], RSTD[:], 0.0, None, ALU.max), reads=[bRSTD], writes=[bRSTD])
                            S.op("act", lambda e: e.activation(RSTD[:], RSTD[:], AF.Sqrt, bias=CST[:, C_EPS:C_EPS + 1]),
                                 reads=[bRSTD, bCST], writes=[bRSTD])
                            S.op("dve", lambda e: e.reciprocal(RSTD[:], RSTD[:]), reads=[bRSTD], writes=[bRSTD])
                            for ec in range(2):
                                c = 2 * hl + ec
                                S.op("dve", lambda e: e.tensor_tensor(YP[:, c * T:(c + 1) * T], YP[:, c * T:(c + 1) * T], mean[:], ALU.subtract),
                                     reads=[bG[4 + c // 4], bmean], writes=[bG[4 + c // 4]])
                                S.op("dve", lambda e: e.tensor_tensor(YP[:, c * T:(c + 1) * T], YP[:, c * T:(c + 1) * T], RSTD[:], ALU.mult),
                                     reads=[bG[4 + c // 4], bRSTD], writes=[bG[4 + c // 4]])
                        for hl in range(4):
                            h = hh * 4 + hl
                            W, bW = ws.pop(base + O_WIN + 24 + h)
                            for ec in range(2):
                                pg, bpg = proj_fm(W, bW, ec)
                                tm, btm = ntmp()
                                S.op("act", lambda e: e.activation(tm[:], pg[:], AF.Silu), reads=[bpg], writes=[btm])
                                c = 2 * hl + ec
                                cg = 2 * h + ec
                                S.op("dve", lambda e: e.scalar_tensor_tensor(
                                    YR[:, cg * T:(cg + 1) * T], YP[:, c * T:(c + 1) * T], VEC[:, vb + 64 + cg: vb + 64 + cg + 1],
                                    tm[:], ALU.mult, ALU.mult), reads=[bG[4 + c // 4], btm, bVEC], writes=[bG[6 + cg // 8]])
                            ws.release()
                    bVG = [bG[0], bG[1], bG[2], bG[3]]
                    S.op("dve", lambda e: e.memset(ST[:], 0.0), writes=[bST])
                    for blk in range(8):
                        proj_v_tm(base + O_WIN + 40 + blk, VG, bVG, blk * 256, 2048, func=AF.Gelu,
                                  accum_col=lambda ch, blk=blk: ch * 8 + blk)
                    for ch in range(NCH):
                        S.op("dve", lambda e: e.tensor_reduce(ST[:, 32 + ch:33 + ch], ST[:, ch * 8:(ch + 1) * 8], AX.X, ALU.add),
                             reads=[bST], writes=[bST])
                        S.op("dve", lambda e: e.tensor_scalar(ST[:, 32 + ch:33 + ch], ST[:, 32 + ch:33 + ch], -1.0 / D, None, ALU.mult),
                             reads=[bST], writes=[bST])
                        S.op("act", lambda e: e.activation(VLN[:, ch * 2048:(ch + 1) * 2048], VG[:, ch * 2048:(ch + 1) * 2048], AF.Square,
                                                           bias=ST[:, 32 + ch:33 + ch], accum_out=ST[:, 36 + ch:37 + ch]),
                             reads=[bVG[ch], bST], writes=[bG[4 + ch // 2], bST])
                        S.op("dve", lambda e: e.tensor_scalar(ST[:, 40 + ch:41 + ch], ST[:, 36 + ch:37 + ch], 1.0 / D, EPS, ALU.mult, ALU.add),
                             reads=[bST], writes=[bST])
                        S.op("act", lambda e: e.activation(ST[:, 40 + ch:41 + ch], ST[:, 40 + ch:41 + ch], AF.Sqrt), reads=[bST], writes=[bST])
                        S.op("dve", lambda e: e.reciprocal(ST[:, 40 + ch:41 + ch], ST[:, 40 + ch:41 + ch]), reads=[bST], writes=[bST])
                        S.op("dve", lambda e: e.tensor_scalar(VLN[:, ch * 2048:(ch + 1) * 2048], VG[:, ch * 2048:(ch + 1) * 2048],
                                                              ST[:, 32 + ch:33 + ch], ST[:, 40 + ch:41 + ch], ALU.add, ALU.mult),
                             reads=[bVG[ch], bST], writes=[bG[4 + ch // 2]])
                    for blk in range(8):
                        W, bW = ws.pop(base + O_WIN + 32 + blk)
                        g = blk
                        for ec in range(2):
                            c = blk * 2 + ec
                            pmx, bpmx = nb()
                            for ch in range(NCH):
                                mm(pmx[:, ch * 128:(ch + 1) * 128], bpmx,
                                   [(VLN[:, ch * 2048 + c * 128: ch * 2048 + (c + 1) * 128], WS[:, g * 128:(g + 1) * 128])],
                                   [bG[4 + ch // 2], bWS])
                            pu, bpu = proj_fm(W, bW, ec)
                            tm, btm = ntmp()
                            S.op("act", lambda e: e.activation(tm[:], pu[:], AF.Gelu), reads=[bpu], writes=[btm])
                            t2, bt2 = ntmp()
                            S.op("dve", lambda e: e.scalar_tensor_tensor(
                                t2[:].rearrange("p (c i) -> p c i", c=4), pmx[:].rearrange("p (c i) -> p c i", c=4),
                                VEC[:, vb + 80 + c: vb + 80 + c + 1],
                                BS[:, g * 128:(g + 1) * 128].unsqueeze(1).to_broadcast([128, 4, 128]), ALU.mult, ALU.add),
                                reads=[bpmx, bVEC, bBS], writes=[bt2])
                            S.op("dve", lambda e: e.tensor_tensor(YS[:, c * T:(c + 1) * T], t2[:], tm[:], ALU.mult),
                                 reads=[bt2, btm], writes=[bG[c // 8]])
                        ws.release()
                    for blk in range(8):
                        Wr, bWr = ws.pop(base + O_BRR + blk)
                        Wsg, bWsg = ws.pop(base + O_BRS + blk)
                        Wgr, bWgr = ws.pop(base + O_WIN + 48 + blk)
                        Wgs, bWgs = ws.pop(base + O_WIN + 56 + blk)
                        for ec in range(2):
                            c = blk * 2 + ec
                            pr, bpr = nb()
                            mm(pr[:], bpr, [(Wr[:, kc * 256 + ec * 128: kc * 256 + ec * 128 + 128], YR[:, kc * T:(kc + 1) * T])
                                            for kc in range(16)], [bWr, bG[6], bG[7]])
                            pss, bpss = nb()
                            mm(pss[:], bpss, [(Wsg[:, kc * 256 + ec * 128: kc * 256 + ec * 128 + 128], YS[:, kc * T:(kc + 1) * T])
                                              for kc in range(16)], [bWsg, bG[0], bG[1]])
                            pgr, bpgr = proj_fm(Wgr, bWgr, ec)
                            pgs, bpgs = proj_fm(Wgs, bWgs, ec)
                            t0, bt0 = TMP[0], bTMP[0]
                            t1, bt1 = TMP[1], bTMP[1]
                            S.op("act", lambda e: e.activation(t0[:], pgr[:], AF.Sigmoid, bias=VEC[:, vb + 32 + c: vb + 32 + c + 1]),
                                 reads=[bpgr, bVEC], writes=[bt0])
                            S.op("act", lambda e: e.activation(t1[:], pgs[:], AF.Sigmoid, bias=VEC[:, vb + 48 + c: vb + 48 + c + 1]),
                                 reads=[bpgs, bVEC], writes=[bt1])
                            S.op("dve", lambda e: e.tensor_tensor(t0[:], t0[:], pr[:], ALU.mult), reads=[bt0, bpr], writes=[bt0])
                            S.op("dve", lambda e: e.tensor_tensor(t1[:], t1[:], pss[:], ALU.mult), reads=[bt1, bpss], writes=[bt1])
                            S.op("dve", lambda e: e.tensor_tensor(MG[:, c * T:(c + 1) * T], t0[:], t1[:], ALU.add),
                                 reads=[bt0, bt1], writes=[bG[2 + c // 8]])
                        ws.release()
                    for blk in range(8):
                        W, bW = ws.pop(base + O_OUT + blk)
                        for ec in range(2):
                            c = blk * 2 + ec
                            po, bpo = nb()
                            mm(po[:], bpo, [(W[:, kc * 256 + ec * 128: kc * 256 + ec * 128 + 128], MG[:, kc * T:(kc + 1) * T])
                                            for kc in range(16)], [bW, bG[2], bG[3]])
                            S.op("dve", lambda e: e.tensor_tensor(X[:, c * T:(c + 1) * T], X[:, c * T:(c + 1) * T], po[:], ALU.add),
                                 reads=[bpo, bX], writes=[bX])
                        ws.release()
                    if DEBUG and l == 0:
                        S.dma("sp", dbgM[t * 128:(t + 1) * 128, :], X[:], sem_out, reads=[bX])
                    rmsnorm(vb + 96)
                    ffn(base, O_GU2, O_DN2)
                    if last:
                        rmsnorm(L * VPL, out_f32=True)
                        S.dma("sp", out_d[t * 128:(t + 1) * 128, :], X[:], sem_out, reads=[bX])
                    else:
                        S.dma("sp", xs[t * 128:(t + 1) * 128, :], X[:], sem_st, reads=[bX], writes=[bXS[t]])
            if not S.dry:
                S.eng["sp"].wait_ge(S.sems[sem_out], S.cnt[sem_out])

        S.dry = True
        program()
        S.dry = False
        psi[0] = 0
        tmi[0] = 0
        program()
        build_nc.last_n_inst = S.n_inst
    return nc


def _blocks_2048(W):
    F = W.shape[1]
    return W.reshape(16, 128, F // 256, 256).transpose(2, 1, 0, 3).reshape(F // 256, 128, 4096)


def _blocks_down(W):
    a = W.reshape(2, 22, 128, 16, 128).transpose(3, 0, 2, 1, 4).reshape(32, 128, 22 * 128)
    out = np.zeros((32, 128, 4096), np.float32)
    out[:, :, :22 * 128] = a
    return out


def _const_table(NTT):
    NT = NTT * T
    lg = gammas()
    cst = np.zeros((128, 2400 + NTT * 32), np.float64)
    j = np.arange(128)[:, None]
    i = np.arange(128)[None, :]
    for h in range(NH):
        cst[:, h * 128:(h + 1) * 128] = np.where(i >= j, np.exp(-(j + 1.0) * lg[h]), 0.0) / 16.0
        cst[:, 1024 + h * 128: 1024 + (h + 1) * 128] = np.exp((i + 1.0) * lg[h])
        cst[:, 2184 + h] = np.exp((127.0 - np.arange(128)) * lg[h]) / 16.0
    cst[:, 2048:2176] = (j <= i)
    cst[:, 2176] = 10000.0 ** (-np.arange(128) / 128.0)
    cst[:, 2193] = EPS
    cst[:, 2208:2336] = np.eye(128)
    for n in range(NT // 128):
        for h in range(NH):
            cst[:, 2400 + n * 8 + h] = np.exp((NT - 1.0 - (n * 128 + np.arange(128))) * lg[h]) / 16.0
    return cst.astype(np.float32)


def prepare(inputs, L, S_total, NTT):
    NT = NTT * T
    x = np.asarray(inputs["x"])
    pos = np.asarray(inputs["positions"])
    blocks = []
    for l in range(L):
        blocks.append(_blocks_2048(np.asarray(inputs["ffn1_w_gu"][l])))
        blocks.append(_blocks_down(np.asarray(inputs["ffn1_w_down"][l])))
        blocks.append(_blocks_2048(np.asarray(inputs["w_in"][l])))
        blocks.append(_blocks_2048(np.asarray(inputs["w_branch_ret"][l])))
        blocks.append(_blocks_2048(np.asarray(inputs["w_branch_sgu"][l])))
        blocks.append(_blocks_2048(np.asarray(inputs["w_out"][l])))
        blocks.append(_blocks_2048(np.asarray(inputs["ffn2_w_gu"][l])))
        blocks.append(_blocks_down(np.asarray(inputs["ffn2_w_down"][l])))
    wsrc = np.concatenate(blocks, axis=0).reshape(L * NBL * 128, BLK)
    del blocks

    def pc(v):
        return np.asarray(v, np.float32).reshape(16, 128).T

    vecs = np.zeros((128, L * VPL + 16), np.float32)
    for l in range(L):
        b = l * VPL
        vecs[:, b + 0:b + 16] = pc(inputs["ffn1_norm"][l])
        vecs[:, b + 16:b + 32] = pc(inputs["mix_norm"][l])
        vecs[:, b + 32:b + 48] = pc(np.asarray(inputs["b_gate"][l])[:D])
        vecs[:, b + 48:b + 64] = pc(np.asarray(inputs["b_gate"][l])[D:])
        vecs[:, b + 64:b + 80] = pc(inputs["ret_gn"][l])
        vecs[:, b + 80:b + 96] = pc(inputs["sgu_ln"][l])
        vecs[:, b + 96:b + 112] = pc(inputs["ffn2_norm"][l])
    vecs[:, L * VPL:] = pc(inputs["final_norm"])
    sguw = np.ascontiguousarray(np.asarray(inputs["sgu_w"], np.float32)[:L].transpose(3, 0, 1, 2)).reshape(128, L * NH * 128)
    sgub = np.asarray(inputs["sgu_b"], np.float32)[:L].reshape(1, L * NH * 128)
    cst = _const_table(NTT)
    in_maps = []
    for c in range(8):
        b, half = c // 2, c % 2
        xc = x[b, half * NT:(half + 1) * NT, :]
        xt = np.ascontiguousarray(xc.reshape(NTT, T, 16, 128).transpose(0, 3, 2, 1)).reshape(NTT * 128, 16 * T)
        cc = cst.copy()
        cc[:, 2192] = float(half)
        in_maps.append({
            "x": xt,
            "pos": np.ascontiguousarray(pos[b, half * NT:(half + 1) * NT].astype(np.int32))[None, :],
            "wsrc": wsrc,
            "vecs": vecs,
            "sguw": sguw,
            "sgub": sgub,
            "cst": cc,
        })
    return in_maps


def assemble(results, B, NTT):
    NT = NTT * T
    out = np.empty((B, 2 * NT, D), np.float32)
    for c in range(8):
        b, half = c // 2, c % 2
        o = np.asarray(results[c]["out"]).reshape(NTT, 128, 16, T).transpose(0, 3, 2, 1).reshape(NT, D)
        out[b, half * NT:(half + 1) * NT, :] = o
    return out


def run(inputs, L, NTT, trace=False):
    nc = build_nc(L, NTT)
    in_maps = prepare(inputs, L, None, NTT)
    res = run_bass_kernel_spmd(nc, in_maps, core_ids=list(range(8)), trace=trace)
    return assemble(res.results, 4, NTT), res


def kernel(**inputs):
    out, _ = run(inputs, 4, 8)
    return out
```

```python
import math
from contextlib import ExitStack

import numpy as np
import concourse.bass as bass
import concourse.mybir as mybir
from concourse.bass_utils import run_bass_kernel_spmd

F32 = mybir.dt.float32
BF16 = mybir.dt.bfloat16
I32 = mybir.dt.int32
AF = mybir.ActivationFunctionType
ALU = mybir.AluOpType
AX = mybir.AxisListType

D = 2048
DFF = 5632
NH = 8
T = 512
NCH = T // 128
EPS = 1e-6
NBL = 240
BLK = 4096
NS = 5
DEBUG = False
VPL = 112
TWO_PI = 2.0 * math.pi
CW1 = 6.28125
CW2 = TWO_PI - CW1

O_GU1, O_DN1, O_WIN, O_BRR, O_BRS, O_OUT, O_GU2, O_DN2 = 0, 44, 76, 140, 148, 156, 164, 208


class Buf:
    __slots__ = ("name", "w", "r")

    def __init__(self, name):
        self.name = name
        self.w = None
        self.r = {}


class Sched:
    def __init__(self, nc, stack):
        self.nc = nc
        self.eng = {"pe": nc.tensor, "act": nc.scalar, "dve": nc.vector,
                    "pool": nc.gpsimd, "sp": nc.sync}
        self.sems = {}
        self.cnt = {}
        self.waited = {e: {} for e in self.eng}
        self.stack = stack
        self.dry = False
        for e in self.eng:
            self.sems["e_" + e] = stack.enter_context(nc.semaphore("sem_" + e))
            self.cnt["e_" + e] = 0
        self.n_inst = 0

    def new_sem(self, name):
        key = "d_" + name
        self.sems[key] = self.stack.enter_context(self.nc.semaphore("sem_" + name))
        self.cnt[key] = 0
        return key

    def _deps(self, reads, writes):
        deps = {}
        for b in reads:
            if b.w is not None:
                k, v = b.w
                if deps.get(k, 0) < v:
                    deps[k] = v
        for b in writes:
            if b.w is not None:
                k, v = b.w
                if deps.get(k, 0) < v:
                    deps[k] = v
            for k, v in b.r.items():
                if deps.get(k, 0) < v:
                    deps[k] = v
        return deps

    def _wait(self, e, deps, embed=True):
        own = "e_" + e
        need = []
        for k, v in deps.items():
            if k == own and (e == "pe" or v > self.cnt[own]):
                continue
            if self.waited[e].get(k, 0) >= v:
                continue
            need.append((k, v))
            self.waited[e][k] = v
        emb = need.pop() if (embed and need) else None
        for k, v in need:
            self.eng[e].wait_ge(self.sems[k], v)
        return emb

    def _mark(self, tok, reads, writes):
        for b in reads:
            if b.r.get(tok[0], 0) < tok[1]:
                b.r[tok[0]] = tok[1]
        for b in writes:
            b.w = tok
            b.r = {}

    def op(self, e, fn, reads=(), writes=(), inc=True):
        if self.dry:
            return None
        emb = self._wait(e, self._deps(reads, writes))
        ins = fn(self.eng[e])
        if emb is not None:
            ins._wait_ge(self.sems[emb[0]], emb[1])
        self.n_inst += 1
        key = "e_" + e
        if inc:
            self.cnt[key] += 1
            ins.then_inc(self.sems[key], 1)
            tok = (key, self.cnt[key])
        else:
            tok = (key, self.cnt[key] + 1)
        self._mark(tok, reads, writes)
        return ins

    def dma(self, q, out, in_, semkey, reads=(), writes=()):
        if self.dry:
            return None
        emb = self._wait(q, self._deps(reads, writes))
        ins = self.eng[q].dma_start(out=out, in_=in_)
        if emb is not None:
            ins._wait_ge(self.sems[emb[0]], emb[1])
        self.n_inst += 1
        self.cnt[semkey] += 16
        ins.then_inc(self.sems[semkey], 16)
        self._mark((semkey, self.cnt[semkey]), reads, writes)
        return ins

    def wait_all(self, e, bufs):
        if self.dry:
            return
        self._wait(e, self._deps(bufs, bufs), embed=False)


def gammas():
    h = np.arange(NH, dtype=np.float64)
    return np.log1p(-np.exp2(-5.0 - h))


def build_nc(L, NTT):
    NT = NTT * T
    NB = L * NBL
    lg = gammas()
    gamma_c = [float(np.exp(128.0 * lg[h])) for h in range(NH)]

    nc = bass.Bass("TRN2", target_bir_lowering=False)
    x_in = nc.dram_tensor("x", [NTT * 128, 16 * T], F32, kind="ExternalInput").ap()
    pos_in = nc.dram_tensor("pos", [1, NT], I32, kind="ExternalInput").ap()
    wsrc = nc.dram_tensor("wsrc", [NB * 128, BLK], F32, kind="ExternalInput").ap()
    vecs_in = nc.dram_tensor("vecs", [128, L * VPL + 16], F32, kind="ExternalInput").ap()
    sguw_in = nc.dram_tensor("sguw", [128, L * NH * 128], F32, kind="ExternalInput").ap()
    sgub_in = nc.dram_tensor("sgub", [1, L * NH * 128], F32, kind="ExternalInput").ap()
    cst_in = nc.dram_tensor("cst", [128, 2400 + NTT * 32], F32, kind="ExternalInput").ap()
    out_d = nc.dram_tensor("out", [NTT * 128, 16 * T], F32, kind="ExternalOutput").ap()
    if DEBUG:
        dbgA = nc.dram_tensor("dbgA", [NTT * 128, 16 * T], F32, kind="ExternalOutput").ap()
        dbgM = nc.dram_tensor("dbgM", [NTT * 128, 16 * T], F32, kind="ExternalOutput").ap()
    xs_t = nc.dram_tensor("xs", [NTT * 128, 16 * T], F32)
    wb_t = [nc.dram_tensor("wb%d" % l, [NBL * 128, BLK], BF16) for l in range(L)]
    cs_t = nc.dram_tensor("cs", [2 * 128, NT], F32)
    cci_t = [nc.dram_tensor("cci%d" % l, [128, 4096], F32) for l in range(L)]
    cco_t = [nc.dram_tensor("cco%d" % l, [256, 4096], F32) for l in range(L)]
    xs = xs_t.ap()
    wbl = [w.ap() for w in wb_t]

    def wb_rows(b):
        return wbl[b // NBL][(b % NBL) * 128:(b % NBL + 1) * 128, :]
    cs = cs_t.ap()

    with ExitStack() as st:
        S = Sched(nc, st)

        def sb(name, shape, dt):
            return st.enter_context(nc.sbuf_tensor(name, shape, dt))

        X = sb("X", [128, 16 * T], F32)
        bX = Buf("X")
        HN = sb("HN", [128, 16 * T], BF16)
        bHN = Buf("HN")
        R = sb("R", [128, 32768], BF16)
        bG = [Buf("g%d" % i) for i in range(8)]
        SL = [sb("SL%d" % i, [128, BLK], BF16) for i in range(NS)]
        bSL = [Buf("SL%d" % i) for i in range(NS)]
        S32 = sb("S32", [128, NH * 512], F32)
        bS32 = [Buf("S32_%d" % h) for h in range(NH)]
        SB = sb("SB", [128, 4 * 512], BF16)
        bSB = [Buf("SB%d" % i) for i in range(4)]
        CS = sb("CS", [128, 2 * T], F32)
        bCS = Buf("CS")
        CST = sb("CST", [128, 2400 + NTT * 32], F32)
        bCST = Buf("CST")
        VEC = sb("VEC", [128, L * VPL + 16], F32)
        bVEC = Buf("VEC")
        WS = sb("WS", [128, NH * 128], BF16)
        bWS = Buf("WS")
        BS = sb("BS", [128, NH * 128], F32)
        bBS = Buf("BS")
        ONEB = sb("ONEB", [128, 128], BF16)
        ONEF = sb("ONEF", [128, 128], F32)
        IDB = sb("IDB", [128, 128], BF16)
        bONE = Buf("ONE")
        TMP = [sb("TMP%d" % i, [128, T], F32) for i in range(3)]
        bTMP = [Buf("TMP%d" % i) for i in range(3)]
        RSTD = sb("RSTD", [128, T], F32)
        bRSTD = Buf("RSTD")
        SQ = [sb("SQ%d" % i, [128, T], BF16) for i in range(2)]
        bSQ = [Buf("SQ%d" % i) for i in range(2)]
        PM = [sb("PM%d" % i, [128, T], BF16) for i in range(2)]
        bPM = [Buf("PM%d" % i) for i in range(2)]
        ST = sb("ST", [128, 64], F32)
        bST = Buf("ST")
        PS = [st.enter_context(nc.psum_tensor("PS%d" % i, [128, 512], F32)) for i in range(8)]
        bPS = [Buf("PS%d" % i) for i in range(8)]
        psi = [0]

        def nb():
            i = psi[0] % 8
            psi[0] += 1
            return PS[i], bPS[i]

        tmi = [0]

        def ntmp():
            i = tmi[0] % 3
            tmi[0] += 1
            return TMP[i], bTMP[i]

        C_MT, C_XI, C_MASK, C_INV, C_ZT, C_FLAG, C_EPS, C_WA = 0, 1024, 2048, 2176, 2184, 2192, 2193, 2400

        def rv_bf(g0, n):
            return R[:, g0 * 4096:g0 * 4096 + n]

        def rv_f32(g0, n):
            return R[:, g0 * 4096:g0 * 4096 + 2 * n].bitcast(F32)

        Hh = rv_bf(0, 44 * T)

        def bH(fc):
            return bG[(fc * T) // 4096]

        sem_w = [S.new_sem("w%d" % i) for i in range(NS)]
        sem_cst = S.new_sem("cst")
        sem_vec = S.new_sem("vec")
        sem_pos = S.new_sem("pos")
        sem_sguw = S.new_sem("sguw")
        sem_bs = S.new_sem("bs")
        sem_cci = S.new_sem("cci")
        sem_s32 = S.new_sem("s32")
        sem_x = S.new_sem("x")
        sem_csl = [S.new_sem("csl0"), S.new_sem("csl1")]
        sem_st = S.new_sem("st")
        sem_pc = [S.new_sem("pc0"), S.new_sem("pc1")]
        sem_pcs = [S.new_sem("pcs%d" % i) for i in range(NS)]
        sem_cs = S.new_sem("cs")
        sem_cc = [S.new_sem("cc%d" % l) for l in range(L)]
        sem_out = S.new_sem("out")
        bXS = [Buf("xs%d" % t) for t in range(NTT)]
        bWB = [Buf("wb%d" % b) for b in range(NB)]
        bCSD = Buf("csd")

        class WStream:
            def __init__(self):
                self.sched = []
                self.i = 0
                self.loaded = 0
                self.released = []
                self.held = []

            def reset(self):
                self.i = 0
                self.loaded = 0
                self.released = [False] * len(self.sched)
                self.held = []

            def _fill(self):
                while (self.loaded < len(self.sched) and self.loaded < self.i + NS
                       and (self.loaded < NS or self.released[self.loaded - NS])):
                    j = self.loaded
                    s = j % NS
                    b = self.sched[j]
                    S.dma("sp", SL[s][:], wb_rows(b), sem_w[s],
                          reads=[bWB[b]], writes=[bSL[s]])
                    self.loaded += 1

            def pop(self, blk):
                if S.dry:
                    self.sched.append(blk)
                    return SL[0], bSL[0]
                i = self.i
                assert self.sched[i] == blk, (i, self.sched[i], blk)
                self._fill()
                assert self.loaded > i, "weight slot ring exhausted (too many blocks held)"
                self.held.append(i)
                self.i += 1
                return SL[i % NS], bSL[i % NS]

            def release(self):
                if S.dry:
                    return
                for i in self.held:
                    self.released[i] = True
                self.held = []
                self._fill()

        ws = WStream()

        def mm(out_ap, bbank, pairs, reads):
            n = len(pairs)
            for i, (l, r) in enumerate(pairs):
                S.op("pe", lambda e: e.matmul(out_ap, l, r, start=(i == 0), stop=(i == n - 1)),
                     reads=reads, writes=[bbank], inc=(i == n - 1))

        def rstd_from(ps_ap, bps, scale):
            S.op("dve", lambda e: e.tensor_scalar(RSTD[:], ps_ap, scale, EPS, ALU.mult, ALU.add),
                 reads=[bps], writes=[bRSTD])
            S.op("act", lambda e: e.activation(RSTD[:], RSTD[:], AF.Sqrt), reads=[bRSTD], writes=[bRSTD])
            S.op("dve", lambda e: e.reciprocal(RSTD[:], RSTD[:]), reads=[bRSTD], writes=[bRSTD])

        def rmsnorm(gcol, out_f32=False):
            pst, bps = nb()
            for c in range(16):
                sq, bsq = SQ[c % 2], bSQ[c % 2]
                S.op("act", lambda e: e.activation(sq[:], X[:, c * T:(c + 1) * T], AF.Square),
                     reads=[bX], writes=[bsq])
                S.op("pe", lambda e: e.matmul(pst[:], ONEB[:], sq[:], start=(c == 0), stop=(c == 15)),
                     reads=[bsq, bONE], writes=[bps], inc=True)
            rstd_from(pst[:], bps, 1.0 / D)
            for c in range(16):
                if out_f32:
                    S.op("dve", lambda e: e.scalar_tensor_tensor(
                        X[:, c * T:(c + 1) * T], X[:, c * T:(c + 1) * T], VEC[:, gcol + c:gcol + c + 1],
                        RSTD[:], ALU.mult, ALU.mult), reads=[bX, bRSTD, bVEC], writes=[bX])
                else:
                    S.op("dve", lambda e: e.scalar_tensor_tensor(
                        HN[:, c * T:(c + 1) * T], X[:, c * T:(c + 1) * T], VEC[:, gcol + c:gcol + c + 1],
                        RSTD[:], ALU.mult, ALU.mult), reads=[bX, bRSTD, bVEC], writes=[bHN])

        def ffn(base, o_gu, o_dn):
            for fb in range(22):
                Wa, bWa = ws.pop(base + o_gu + fb)
                Wg, bWg = ws.pop(base + o_gu + 22 + fb)
                for fc in range(2):
                    pa, bpa = nb()
                    pg, bpg = nb()
                    mm(pa[:], bpa, [(Wa[:, kc * 256 + fc * 128: kc * 256 + fc * 128 + 128], HN[:, kc * T:(kc + 1) * T])
                                    for kc in range(16)], [bWa, bHN])
                    mm(pg[:], bpg, [(Wg[:, kc * 256 + fc * 128: kc * 256 + fc * 128 + 128], HN[:, kc * T:(kc + 1) * T])
                                    for kc in range(16)], [bWg, bHN])
                    tm, btm = ntmp()
                    S.op("act", lambda e: e.activation(tm[:], pg[:], AF.Silu), reads=[bpg], writes=[btm])
                    f = fb * 2 + fc
                    S.op("dve", lambda e: e.tensor_tensor(Hh[:, f * T:(f + 1) * T], pa[:], tm[:], ALU.mult),
                         reads=[bpa, btm], writes=[bH(f)])
                ws.release()
            for dc in range(16):
                po, bpo = nb()
                for half in range(2):
                    Wd, bWd = ws.pop(base + o_dn + dc * 2 + half)
                    for fl in range(22):
                        f = half * 22 + fl
                        S.op("pe", lambda e: e.matmul(po[:], Wd[:, fl * 128:(fl + 1) * 128], Hh[:, f * T:(f + 1) * T],
                                                      start=(f == 0), stop=(f == 43)),
                             reads=[bWd, bH(f)], writes=[bpo], inc=(fl == 21))
                    ws.release()
                S.op("dve", lambda e: e.scalar_tensor_tensor(
                    X[:, dc * T:(dc + 1) * T], po[:], 0.5, X[:, dc * T:(dc + 1) * T], ALU.mult, ALU.add),
                    reads=[bpo, bX], writes=[bX])

        def rotary(pa, bpa, pb, bpb, dst1, dst2, bdst):
            cos = CS[:, 0:T]
            sin = CS[:, T:2 * T]
            t0, bt0 = TMP[0], bTMP[0]
            t1, bt1 = TMP[1], bTMP[1]
            S.op("dve", lambda e: e.tensor_tensor(t0[:], pa[:], cos, ALU.mult), reads=[bpa, bCS], writes=[bt0])
            S.op("dve", lambda e: e.tensor_tensor(t1[:], pb[:], sin, ALU.mult), reads=[bpb, bCS], writes=[bt1])
            S.op("dve", lambda e: e.tensor_tensor(dst1, t0[:], t1[:], ALU.subtract), reads=[bt0, bt1], writes=[bdst])
            S.op("dve", lambda e: e.tensor_tensor(t0[:], pa[:], sin, ALU.mult), reads=[bpa, bCS], writes=[bt0])
            S.op("dve", lambda e: e.tensor_tensor(t1[:], pb[:], cos, ALU.mult), reads=[bpb, bCS], writes=[bt1])
            S.op("dve", lambda e: e.tensor_tensor(dst2, t0[:], t1[:], ALU.add), reads=[bt0, bt1], writes=[bdst])

        def proj_fm(W, bW, fc):
            p, bp = nb()
            mm(p[:], bp, [(W[:, kc * 256 + fc * 128: kc * 256 + fc * 128 + 128], HN[:, kc * T:(kc + 1) * T])
                          for kc in range(16)], [bW, bHN])
            return p, bp

        def proj_head_rot(blk, dst, bdst, hl):
            W, bW = ws.pop(blk)
            pa, bpa = proj_fm(W, bW, 0)
            pb, bpb = proj_fm(W, bW, 1)
            ws.release()
            rotary(pa, bpa, pb, bpb, dst[:, (2 * hl) * T:(2 * hl + 1) * T], dst[:, (2 * hl + 1) * T:(2 * hl + 2) * T], bdst)

        def proj_v_tm(blk, dst, bdst, col0, width_total, func=None, accum_col=None):
            W, bW = ws.pop(blk)
            for ch in range(NCH):
                p, bp = nb()
                mm(p[:, 0:256], bp, [(HN[:, kc * T + ch * 128: kc * T + ch * 128 + 128], W[:, kc * 256:(kc + 1) * 256])
                                     for kc in range(16)], [bW, bHN])
                o = dst[:, ch * width_total + col0: ch * width_total + col0 + 256]
                if func is None:
                    S.op("act", lambda e: e.activation(o, p[:, 0:256], AF.Copy), reads=[bp], writes=[bdst[ch]])
                else:
                    S.op("act", lambda e: e.activation(o, p[:, 0:256], func, accum_out=ST[:, accum_col(ch):accum_col(ch) + 1]),
                         reads=[bp], writes=[bdst[ch], bST])
            ws.release()

        def program():
            ws.reset()
            S.dma("sp", CST[:], cst_in, sem_cst, writes=[bCST])
            S.dma("sp", VEC[:], vecs_in, sem_vec, writes=[bVEC])
            S.op("dve", lambda e: e.memset(ONEF[:], 1.0), writes=[bONE])
            S.op("dve", lambda e: e.tensor_copy(ONEB[:], ONEF[:]), reads=[bONE], writes=[bONE])
            S.op("dve", lambda e: e.tensor_copy(IDB[:], CST[:, 2208:2336]), reads=[bCST, bONE], writes=[bONE])
            for t in range(NTT):
                PI_, bPI = TMP[2], bTMP[2]
                pi_i = PI_[:].bitcast(I32)
                S.dma("sp", pi_i, pos_in[:, t * T:(t + 1) * T].partition_broadcast(128), sem_pos, writes=[bPI])
                ang, bang = TMP[0], bTMP[0]
                S.op("dve", lambda e: e.tensor_copy(ang[:], pi_i), reads=[bPI], writes=[bang])
                S.op("dve", lambda e: e.tensor_scalar(ang[:], ang[:], CST[:, C_INV:C_INV + 1], None, ALU.mult),
                     reads=[bang, bCST], writes=[bang])
                for which in range(2):
                    a2, ba2 = TMP[1], bTMP[1]
                    shift = (math.pi / 2.0) if which == 0 else 0.0
                    S.op("dve", lambda e: e.tensor_scalar(a2[:], ang[:], shift, 1.0 / TWO_PI, ALU.add, ALU.mult),
                         reads=[bang], writes=[ba2])
                    S.op("dve", lambda e: e.tensor_copy(pi_i, a2[:]), reads=[ba2], writes=[bPI])
                    S.op("dve", lambda e: e.tensor_copy(a2[:], pi_i), reads=[bPI], writes=[ba2])
                    r, br = RSTD, bRSTD
                    S.op("dve", lambda e: e.scalar_tensor_tensor(r[:], a2[:], -CW1, ang[:], ALU.mult, ALU.add),
                         reads=[ba2, bang], writes=[br])
                    S.op("dve", lambda e: e.scalar_tensor_tensor(r[:], a2[:], -CW2, r[:], ALU.mult, ALU.add),
                         reads=[ba2, br], writes=[br])
                    S.op("dve", lambda e: e.tensor_scalar(r[:], r[:], shift, None, ALU.add), reads=[br], writes=[br])
                    S.op("dve", lambda e: e.tensor_scalar(r[:], r[:], math.pi, -math.pi, ALU.min, ALU.max),
                         reads=[br], writes=[br])
                    S.op("act", lambda e: e.activation(CS[:, which * T:(which + 1) * T], r[:], AF.Sin),
                         reads=[br], writes=[bCS])
                for which in range(2):
                    S.dma("sp", cs[which * 128:(which + 1) * 128, t * T:(t + 1) * T], CS[:, which * T:(which + 1) * T],
                          sem_cs, reads=[bCS], writes=[bCSD])
            STG = [X[:, 0:BLK], X[:, BLK:2 * BLK]]
            bSTG = [Buf("stg0"), Buf("stg1")]
            S.wait_all("sp", [bX])
            for b in range(NB):
                sgi = b % 2
                sl = b % NS
                S.dma("sp", STG[sgi], wsrc[b * 128:(b + 1) * 128, :], sem_pc[sgi], writes=[bSTG[sgi], bX])
                if b % 2 == 0:
                    S.op("act", lambda e: e.activation(SL[sl][:], STG[sgi], AF.Copy), reads=[bSTG[sgi], bX], writes=[bSL[sl]])
                else:
                    S.op("dve", lambda e: e.tensor_copy(SL[sl][:], STG[sgi]), reads=[bSTG[sgi], bX], writes=[bSL[sl]])
                S.dma("sp", wb_rows(b), SL[sl][:], sem_pcs[sl], reads=[bSL[sl]], writes=[bWB[b]])

            for l in range(L):
                base = l * NBL
                vb = l * VPL
                tmw, btmw = TMP[2], bTMP[2]
                for half in range(2):
                    S.dma("sp", tmw[:], sguw_in[:, (l * NH + half * 4) * 128:(l * NH + half * 4 + 4) * 128], sem_sguw, writes=[btmw])
                    S.op("dve", lambda e: e.tensor_tensor(
                        WS[:, half * 512:(half + 1) * 512].rearrange("p (g i) -> p g i", g=4),
                        tmw[:].rearrange("p (g i) -> p g i", g=4),
                        CST[:, C_MASK:C_MASK + 128].unsqueeze(1).to_broadcast([128, 4, 128]), ALU.mult),
                        reads=[btmw, bCST], writes=[bWS])
                S.dma("sp", BS[:], sgub_in[:, l * NH * 128:(l + 1) * NH * 128].partition_broadcast(128), sem_bs, writes=[bBS])
                for h in range(NH):
                    S.op("dve", lambda e: e.memset(S32[:, h * 512:(h + 1) * 512], 0.0), writes=[bS32[h]])

                KT = rv_bf(0, 16 * T)
                bKT = Buf("KTa")
                KW = rv_bf(2, 4 * 2048)
                VV = rv_bf(4, 4 * 2048)
                for t in range(NTT):
                    src = x_in if l == 0 else xs
                    S.dma("sp", X[:], src[t * 128:(t + 1) * 128, :], sem_x, reads=[bXS[t]], writes=[bX])
                    for which in range(2):
                        S.dma("sp", CS[:, which * T:(which + 1) * T], cs[which * 128:(which + 1) * 128, t * T:(t + 1) * T],
                              sem_csl[which], reads=[bCSD], writes=[bCS])
                    rmsnorm(vb + 0)
                    ffn(base, O_GU1, O_DN1)
                    S.dma("sp", xs[t * 128:(t + 1) * 128, :], X[:], sem_st, reads=[bX], writes=[bXS[t]])
                    if DEBUG and l == 0:
                        S.dma("sp", dbgA[t * 128:(t + 1) * 128, :], X[:], sem_out, reads=[bX])
                    rmsnorm(vb + 16)
                    bk = [bG[0], bG[1]]
                    for h in range(NH):
                        W, bW = ws.pop(base + O_WIN + 8 + h)
                        pa, bpa = proj_fm(W, bW, 0)
                        pb, bpb = proj_fm(W, bW, 1)
                        ws.release()
                        rotary(pa, bpa, pb, bpb, KT[:, (2 * h) * T:(2 * h + 1) * T], KT[:, (2 * h + 1) * T:(2 * h + 2) * T], bk[h // 4])
                    for h in range(NH):
                        proj_v_tm(base + O_WIN + 16 + h, VV, [bG[4], bG[4], bG[5], bG[5]], h * 256, 2048)
                    bKTall = Buf("x")

                    def tabA(ch, h0, hcnt, t=t):
                        n = t * NCH + ch
                        return CST[:, C_WA + n * 8 + h0: C_WA + n * 8 + h0 + hcnt].unsqueeze(2).to_broadcast([128, hcnt, 256])
                    for ch in range(NCH):
                        for half in range(2):
                            p, bp = nb()
                            pbv = p[:].bitcast(BF16)
                            for c8 in range(8):
                                c = half * 8 + c8
                                S.op("pe", lambda e: e.transpose(pbv[:, c8 * 128:(c8 + 1) * 128],
                                                                 KT[:, c * T + ch * 128: c * T + ch * 128 + 128], IDB[:]),
                                     reads=[bk[half], bONE], writes=[bp], inc=(c8 == 7))
                            o = KW[:, ch * 2048 + half * 1024: ch * 2048 + half * 1024 + 1024]
                            S.op("dve", lambda e: e.tensor_tensor(
                                o.rearrange("p (h d) -> p h d", h=4),
                                pbv[:, 0:1024].rearrange("p (h d) -> p h d", h=4),
                                tabA(ch, half * 4, 4), ALU.mult), reads=[bp, bCST], writes=[bG[2 + ch // 2]])
                    for h in range(NH):
                        pu, bpu = nb()
                        for dc in range(2):
                            mm(pu[:, dc * 256:(dc + 1) * 256], bpu,
                               [(KW[:, ch * 2048 + h * 256 + dc * 128: ch * 2048 + h * 256 + dc * 128 + 128],
                                 VV[:, ch * 2048 + h * 256: ch * 2048 + (h + 1) * 256]) for ch in range(NCH)],
                               [bG[2], bG[3], bG[4], bG[5]])
                        S.op("dve", lambda e: e.tensor_tensor(S32[:, h * 512:(h + 1) * 512], S32[:, h * 512:(h + 1) * 512],
                                                              pu[:], ALU.add), reads=[bpu, bS32[h]], writes=[bS32[h]])

                bcci = Buf("cci")
                bcco = Buf("cco")
                S.dma("pool", cci_t[l].ap(), S32[:], sem_cci, reads=bS32, writes=[bcci])
                S.wait_all("pool", [bcci])
                if not S.dry:
                    ins = nc.gpsimd.collective_compute("AllGather", ALU.bypass,
                                                       replica_groups=[[0, 1], [2, 3], [4, 5], [6, 7]],
                                                       ins=[cci_t[l].ap().opt()], outs=[cco_t[l].ap().opt()])
                    S.cnt[sem_cc[l]] += 1
                    ins.then_inc(S.sems[sem_cc[l]], 1)
                    bcco.w = (sem_cc[l], 1)
                S.dma("sp", S32[:], cco_t[l].ap()[0:128, :], sem_s32, reads=[bcco], writes=bS32)
                for h in range(NH):
                    S.op("dve", lambda e: e.tensor_scalar(S32[:, h * 512:(h + 1) * 512], S32[:, h * 512:(h + 1) * 512],
                                                          CST[:, C_FLAG:C_FLAG + 1], None, ALU.mult),
                         reads=[bS32[h], bCST], writes=[bS32[h]])

                QT = rv_bf(0, 8 * T)
                KT2 = rv_bf(1, 8 * T)
                KZ = rv_bf(2, 4 * 1024)
                V4 = rv_bf(3, 4 * 1024)
                YP = rv_f32(4, 8 * T)
                YR = rv_bf(6, 16 * T)
                VG = rv_f32(0, 4 * 2048)
                VLN = rv_bf(4, 4 * 2048)
                YS = rv_bf(0, 16 * T)
                MG = rv_bf(2, 16 * T)
                last = (l == L - 1)
                for t in range(NTT):
                    S.dma("sp", X[:], xs[t * 128:(t + 1) * 128, :], sem_x, reads=[bXS[t]], writes=[bX])
                    for which in range(2):
                        S.dma("sp", CS[:, which * T:(which + 1) * T], cs[which * 128:(which + 1) * 128, t * T:(t + 1) * T],
                              sem_csl[which], reads=[bCSD], writes=[bCS])
                    rmsnorm(vb + 16)
                    for hh in range(2):
                        for hl in range(4):
                            h = hh * 4 + hl
                            proj_head_rot(base + O_WIN + h, QT, bG[0], hl)
                        for hl in range(4):
                            h = hh * 4 + hl
                            proj_head_rot(base + O_WIN + 8 + h, KT2, bG[1], hl)
                        for hl in range(4):
                            h = hh * 4 + hl
                            proj_v_tm(base + O_WIN + 16 + h, V4, [bG[3]] * 4, hl * 256, 1024)
                        for ch in range(NCH):
                            p, bp = nb()
                            pbv = p[:].bitcast(BF16)
                            for c in range(8):
                                S.op("pe", lambda e: e.transpose(pbv[:, c * 128:(c + 1) * 128],
                                                                 KT2[:, c * T + ch * 128: c * T + ch * 128 + 128], IDB[:]),
                                     reads=[bG[1], bONE], writes=[bp], inc=(c == 7))
                            S.op("dve", lambda e: e.tensor_tensor(
                                KZ[:, ch * 1024:(ch + 1) * 1024].rearrange("p (h d) -> p h d", h=4),
                                pbv[:, 0:1024].rearrange("p (h d) -> p h d", h=4),
                                CST[:, C_ZT + hh * 4: C_ZT + hh * 4 + 4].unsqueeze(2).to_broadcast([128, 4, 256]), ALU.mult),
                                reads=[bp, bCST], writes=[bG[2]])
                        for hl in range(4):
                            h = hh * 4 + hl
                            U = []
                            for ch in range(NCH):
                                pu, bpu = nb()
                                for dc in range(2):
                                    mm(pu[:, dc * 256:(dc + 1) * 256], bpu,
                                       [(KZ[:, ch * 1024 + hl * 256 + dc * 128: ch * 1024 + hl * 256 + dc * 128 + 128],
                                         V4[:, ch * 1024 + hl * 256: ch * 1024 + (hl + 1) * 256])], [bG[2], bG[3]])
                                U.append((pu, bpu))
                            psc, bpsc = nb()
                            for ch in range(NCH):
                                mm(psc[:, ch * 128:(ch + 1) * 128], bpsc,
                                   [(KT2[:, (2 * hl + dc) * T + ch * 128: (2 * hl + dc) * T + ch * 128 + 128],
                                     QT[:, (2 * hl + dc) * T + ch * 128: (2 * hl + dc) * T + ch * 128 + 128]) for dc in range(2)],
                                   [bG[0], bG[1]])
                            pm, bpm = PM[h % 2], bPM[h % 2]
                            S.op("dve", lambda e: e.tensor_tensor(
                                pm[:].rearrange("p (c i) -> p c i", c=4), psc[:].rearrange("p (c i) -> p c i", c=4),
                                CST[:, C_MT + h * 128: C_MT + (h + 1) * 128].unsqueeze(1).to_broadcast([128, 4, 128]), ALU.mult),
                                reads=[bpsc, bCST], writes=[bpm])
                            for ch in range(NCH):
                                S.op("act", lambda e: e.activation(SB[:, ch * 512:(ch + 1) * 512], S32[:, h * 512:(h + 1) * 512], AF.Copy),
                                     reads=[bS32[h]], writes=[bSB[ch]])
                                pu, bpu = U[ch]
                                S.op("dve", lambda e: e.scalar_tensor_tensor(
                                    S32[:, h * 512:(h + 1) * 512], S32[:, h * 512:(h + 1) * 512], gamma_c[h], pu[:], ALU.mult, ALU.add),
                                    reads=[bS32[h], bpu], writes=[bS32[h]])
                            for ec in range(2):
                                py, bpy = nb()
                                for ch in range(NCH):
                                    prs = [(V4[:, ch * 1024 + hl * 256 + ec * 128: ch * 1024 + hl * 256 + ec * 128 + 128],
                                            pm[:, ch * 128:(ch + 1) * 128])]
                                    for dc in range(2):
                                        prs.append((SB[:, ch * 512 + dc * 256 + ec * 128: ch * 512 + dc * 256 + ec * 128 + 128],
                                                    QT[:, (2 * hl + dc) * T + ch * 128: (2 * hl + dc) * T + ch * 128 + 128]))
                                    mm(py[:, ch * 128:(ch + 1) * 128], bpy, prs, [bG[3], bpm, bSB[ch], bG[0]])
                                c = 2 * hl + ec
                                S.op("dve", lambda e: e.tensor_tensor(
                                    YP[:, c * T:(c + 1) * T].rearrange("p (c i) -> p c i", c=4), py[:].rearrange("p (c i) -> p c i", c=4),
                                    CST[:, C_XI + h * 128: C_XI + (h + 1) * 128].unsqueeze(1).to_broadcast([128, 4, 128]), ALU.mult),
                                    reads=[bpy, bCST], writes=[bG[4 + c // 4]])
                            p1, bp1 = nb()
                            p2, bp2 = nb()
                            sqs = []
                            for ec in range(2):
                                c = 2 * hl + ec
                                tq, btq = TMP[ec], bTMP[ec]
                                S.op("act", lambda e: e.activation(tq[:], YP[:, c * T:(c + 1) * T], AF.Square),
                                     reads=[bG[4 + c // 4]], writes=[btq])
                                sqs.append((tq, btq))
                            for ec in range(2):
                                c = 2 * hl + ec
                                S.op("pe", lambda e: e.matmul(p1[:], ONEF[:], YP[:, c * T:(c + 1) * T], start=(ec == 0), stop=(ec == 1)),
                                     reads=[bG[4 + c // 4], bONE], writes=[bp1], inc=(ec == 1))
                            for ec in range(2):
                                tq, btq = sqs[ec]
                                S.op("pe", lambda e: e.matmul(p2[:], ONEF[:], tq[:], start=(ec == 0), stop=(ec == 1)),
                                     reads=[btq, bONE], writes=[bp2], inc=(ec == 1))
                            mean, bmean = TMP[2], bTMP[2]
                            S.op("dve", lambda e: e.tensor_scalar(mean[:], p1[:], 1.0 / 256.0, None, ALU.mult), reads=[bp1], writes=[bmean])
                            m2, bm2 = TMP[0], bTMP[0]
                            S.op("act", lambda e: e.activation(m2[:], p1[:], AF.Square, scale=1.0 / 256.0), reads=[bp1], writes=[bm2])
                            S.op("dve", lambda e: e.scalar_tensor_tensor(RSTD[:], p2[:], 1.0 / 256.0, m2[:], ALU.mult, ALU.subtract),
                                 reads=[bp2, bm2], writes=[bRSTD])
                            S.op("dve", lambda e: e.tensor_scalar(RSTD[:], RSTD[:], 0.0, None, ALU.max), reads=[bRSTD], writes=[bRSTD])
                            S.op("act", lambda e: e.activation(RSTD[:], RSTD[:], AF.Sqrt, bias=CST[:, C_EPS:C_EPS + 1]),
                                 reads=[bRSTD, bCST], writes=[bRSTD])
                            S.op("dve", lambda e: e.reciprocal(RSTD[:], RSTD[:]), reads=[bRSTD], writes=[bRSTD])
                            for ec in range(2):
                                c = 2 * hl + ec
                                S.op("dve", lambda e: e.tensor_tensor(YP[:, c * T:(c + 1) * T], YP[:, c * T:(c + 1) * T], mean[:], ALU.subtract),
                                     reads=[bG[4 + c // 4], bmean], writes=[bG[4 + c // 4]])
                                S.op("dve", lambda e: e.tensor_tensor(YP[:, c * T:(c + 1) * T], YP[:, c * T:(c + 1) * T], RSTD[:], ALU.mult),
                                     reads=[bG[4 + c // 4], bRSTD], writes=[bG[4 + c // 4]])
                        for hl in range(4):
                            h = hh * 4 + hl
                            W, bW = ws.pop(base + O_WIN + 24 + h)
                            for ec in range(2):
                                pg, bpg = proj_fm(W, bW, ec)
                                tm, btm = ntmp()
                                S.op("act", lambda e: e.activation(tm[:], pg[:], AF.Silu), reads=[bpg], writes=[btm])
                                c = 2 * hl + ec
                                cg = 2 * h + ec
                                S.op("dve", lambda e: e.scalar_tensor_tensor(
                                    YR[:, cg * T:(cg + 1) * T], YP[:, c * T:(c + 1) * T], VEC[:, vb + 64 + cg: vb + 64 + cg + 1],
                                    tm[:], ALU.mult, ALU.mult), reads=[bG[4 + c // 4], btm, bVEC], writes=[bG[6 + cg // 8]])
                            ws.release()
                    bVG = [bG[0], bG[1], bG[2], bG[3]]
                    S.op("dve", lambda e: e.memset(ST[:], 0.0), writes=[bST])
                    for blk in range(8):
                        proj_v_tm(base + O_WIN + 40 + blk, VG, bVG, blk * 256, 2048, func=AF.Gelu,
                                  accum_col=lambda ch, blk=blk: ch * 8 + blk)
                    for ch in range(NCH):
                        S.op("dve", lambda e: e.tensor_reduce(ST[:, 32 + ch:33 + ch], ST[:, ch * 8:(ch + 1) * 8], AX.X, ALU.add),
                             reads=[bST], writes=[bST])
                        S.op("dve", lambda e: e.tensor_scalar(ST[:, 32 + ch:33 + ch], ST[:, 32 + ch:33 + ch], -1.0 / D, None, ALU.mult),
                             reads=[bST], writes=[bST])
                        S.op("act", lambda e: e.activation(VLN[:, ch * 2048:(ch + 1) * 2048], VG[:, ch * 2048:(ch + 1) * 2048], AF.Square,
                                                           bias=ST[:, 32 + ch:33 + ch], accum_out=ST[:, 36 + ch:37 + ch]),
                             reads=[bVG[ch], bST], writes=[bG[4 + ch // 2], bST])
                        S.op("dve", lambda e: e.tensor_scalar(ST[:, 40 + ch:41 + ch], ST[:, 36 + ch:37 + ch], 1.0 / D, EPS, ALU.mult, ALU.add),
                             reads=[bST], writes=[bST])
                        S.op("act", lambda e: e.activation(ST[:, 40 + ch:41 + ch], ST[:, 40 + ch:41 + ch], AF.Sqrt), reads=[bST], writes=[bST])
                        S.op("dve", lambda e: e.reciprocal(ST[:, 40 + ch:41 + ch], ST[:, 40 + ch:41 + ch]), reads=[bST], writes=[bST])
                        S.op("dve", lambda e: e.tensor_scalar(VLN[:, ch * 2048:(ch + 1) * 2048], VG[:, ch * 2048:(ch + 1) * 2048],
                                                              ST[:, 32 + ch:33 + ch], ST[:, 40 + ch:41 + ch], ALU.add, ALU.mult),
                             reads=[bVG[ch], bST], writes=[bG[4 + ch // 2]])
                    for blk in range(8):
                        W, bW = ws.pop(base + O_WIN + 32 + blk)
                        g = blk
                        for ec in range(2):
                            c = blk * 2 + ec
                            pmx, bpmx = nb()
                            for ch in range(NCH):
                                mm(pmx[:, ch * 128:(ch + 1) * 128], bpmx,
                                   [(VLN[:, ch * 2048 + c * 128: ch * 2048 + (c + 1) * 128], WS[:, g * 128:(g + 1) * 128])],
                                   [bG[4 + ch // 2], bWS])
                            pu, bpu = proj_fm(W, bW, ec)
                            tm, btm = ntmp()
                            S.op("act", lambda e: e.activation(tm[:], pu[:], AF.Gelu), reads=[bpu], writes=[btm])
                            t2, bt2 = ntmp()
                            S.op("dve", lambda e: e.scalar_tensor_tensor(
                                t2[:].rearrange("p (c i) -> p c i", c=4), pmx[:].rearrange("p (c i) -> p c i", c=4),
                                VEC[:, vb + 80 + c: vb + 80 + c + 1],
                                BS[:, g * 128:(g + 1) * 128].unsqueeze(1).to_broadcast([128, 4, 128]), ALU.mult, ALU.add),
                                reads=[bpmx, bVEC, bBS], writes=[bt2])
                            S.op("dve", lambda e: e.tensor_tensor(YS[:, c * T:(c + 1) * T], t2[:], tm[:], ALU.mult),
                                 reads=[bt2, btm], writes=[bG[c // 8]])
                        ws.release()
                    for blk in range(8):
                        Wr, bWr = ws.pop(base + O_BRR + blk)
                        Wsg, bWsg = ws.pop(base + O_BRS + blk)
                        Wgr, bWgr = ws.pop(base + O_WIN + 48 + blk)
                        Wgs, bWgs = ws.pop(base + O_WIN + 56 + blk)
                        for ec in range(2):
                            c = blk * 2 + ec
                            pr, bpr = nb()
                            mm(pr[:], bpr, [(Wr[:, kc * 256 + ec * 128: kc * 256 + ec * 128 + 128], YR[:, kc * T:(kc + 1) * T])
                                            for kc in range(16)], [bWr, bG[6], bG[7]])
                            pss, bpss = nb()
                            mm(pss[:], bpss, [(Wsg[:, kc * 256 + ec * 128: kc * 256 + ec * 128 + 128], YS[:, kc * T:(kc + 1) * T])
                                              for kc in range(16)], [bWsg, bG[0], bG[1]])
                            pgr, bpgr = proj_fm(Wgr, bWgr, ec)
                            pgs, bpgs = proj_fm(Wgs, bWgs, ec)
                            t0, bt0 = TMP[0], bTMP[0]
                            t1, bt1 = TMP[1], bTMP[1]
                            S.op("act", lambda e: e.activation(t0[:], pgr[:], AF.Sigmoid, bias=VEC[:, vb + 32 + c: vb + 32 + c + 1]),
                                 reads=[bpgr, bVEC], writes=[bt0])
                            S.op("act", lambda e: e.activation(t1[:], pgs[:], AF.Sigmoid, bias=VEC[:, vb + 48 + c: vb + 48 + c + 1]),
                                 reads=[bpgs, bVEC], writes=[bt1])
                            S.op("dve", lambda e: e.tensor_tensor(t0[:], t0[:], pr[:], ALU.mult), reads=[bt0, bpr], writes=[bt0])
                            S.op("dve", lambda e: e.tensor_tensor(t1[:], t1[:], pss[:], ALU.mult), reads=[bt1, bpss], writes=[bt1])
                            S.op("dve", lambda e: e.tensor_tensor(MG[:, c * T:(c + 1) * T], t0[:], t1[:], ALU.add),
                                 reads=[bt0, bt1], writes=[bG[2 + c // 8]])
                        ws.release()
                    for blk in range(8):
                        W, bW = ws.pop(base + O_OUT + blk)
                        for ec in range(2):
                            c = blk * 2 + ec
                            po, bpo = nb()
                            mm(po[:], bpo, [(W[:, kc * 256 + ec * 128: kc * 256 + ec * 128 + 128], MG[:, kc * T:(kc + 1) * T])
                                            for kc in range(16)], [bW, bG[2], bG[3]])
                            S.op("dve", lambda e: e.tensor_tensor(X[:, c * T:(c + 1) * T], X[:, c * T:(c + 1) * T], po[:], ALU.add),
                                 reads=[bpo, bX], writes=[bX])
                        ws.release()
                    if DEBUG and l == 0:
                        S.dma("sp", dbgM[t * 128:(t + 1) * 128, :], X[:], sem_out, reads=[bX])
                    rmsnorm(vb + 96)
                    ffn(base, O_GU2, O_DN2)
                    if last:
                        rmsnorm(L * VPL, out_f32=True)
                        S.dma("sp", out_d[t * 128:(t + 1) * 128, :], X[:], sem_out, reads=[bX])
                    else:
                        S.dma("sp", xs[t * 128:(t + 1) * 128, :], X[:], sem_st, reads=[bX], writes=[bXS[t]])
            if not S.dry:
                S.eng["sp"].wait_ge(S.sems[sem_out], S.cnt[sem_out])

        S.dry = True
        program()
        S.dry = False
        psi[0] = 0
        tmi[0] = 0
        program()
        build_nc.last_n_inst = S.n_inst
    return nc


def _blocks_2048(W):
    F = W.shape[1]
    return W.reshape(16, 128, F // 256, 256).transpose(2, 1, 0, 3).reshape(F // 256, 128, 4096)


def _blocks_down(W):
    a = W.reshape(2, 22, 128, 16, 128).transpose(3, 0, 2, 1, 4).reshape(32, 128, 22 * 128)
    out = np.zeros((32, 128, 4096), np.float32)
    out[:, :, :22 * 128] = a
    return out


def _const_table(NTT):
    NT = NTT * T
    lg = gammas()
    cst = np.zeros((128, 2400 + NTT * 32), np.float64)
    j = np.arange(128)[:, None]
    i = np.arange(128)[None, :]
    for h in range(NH):
        cst[:, h * 128:(h + 1) * 128] = np.where(i >= j, np.exp(-(j + 1.0) * lg[h]), 0.0) / 16.0
        cst[:, 1024 + h * 128: 1024 + (h + 1) * 128] = np.exp((i + 1.0) * lg[h])
        cst[:, 2184 + h] = np.exp((127.0 - np.arange(128)) * lg[h]) / 16.0
    cst[:, 2048:2176] = (j <= i)
    cst[:, 2176] = 10000.0 ** (-np.arange(128) / 128.0)
    cst[:, 2193] = EPS
    cst[:, 2208:2336] = np.eye(128)
    for n in range(NT // 128):
        for h in range(NH):
            cst[:, 2400 + n * 8 + h] = np.exp((NT - 1.0 - (n * 128 + np.arange(128))) * lg[h]) / 16.0
    return cst.astype(np.float32)


def prepare(inputs, L, S_total, NTT):
    NT = NTT * T
    x = np.asarray(inputs["x"])
    pos = np.asarray(inputs["positions"])
    blocks = []
    for l in range(L):
        blocks.append(_blocks_2048(np.asarray(inputs["ffn1_w_gu"][l])))
        blocks.append(_blocks_down(np.asarray(inputs["ffn1_w_down"][l])))
        blocks.append(_blocks_2048(np.asarray(inputs["w_in"][l])))
        blocks.append(_blocks_2048(np.asarray(inputs["w_branch_ret"][l])))
        blocks.append(_blocks_2048(np.asarray(inputs["w_branch_sgu"][l])))
        blocks.append(_blocks_2048(np.asarray(inputs["w_out"][l])))
        blocks.append(_blocks_2048(np.asarray(inputs["ffn2_w_gu"][l])))
        blocks.append(_blocks_down(np.asarray(inputs["ffn2_w_down"][l])))
    wsrc = np.concatenate(blocks, axis=0).reshape(L * NBL * 128, BLK)
    del blocks

    def pc(v):
        return np.asarray(v, np.float32).reshape(16, 128).T

    vecs = np.zeros((128, L * VPL + 16), np.float32)
    for l in range(L):
        b = l * VPL
        vecs[:, b + 0:b + 16] = pc(inputs["ffn1_norm"][l])
        vecs[:, b + 16:b + 32] = pc(inputs["mix_norm"][l])
        vecs[:, b + 32:b + 48] = pc(np.asarray(inputs["b_gate"][l])[:D])
        vecs[:, b + 48:b + 64] = pc(np.asarray(inputs["b_gate"][l])[D:])
        vecs[:, b + 64:b + 80] = pc(inputs["ret_gn"][l])
        vecs[:, b + 80:b + 96] = pc(inputs["sgu_ln"][l])
        vecs[:, b + 96:b + 112] = pc(inputs["ffn2_norm"][l])
    vecs[:, L * VPL:] = pc(inputs["final_norm"])
    sguw = np.ascontiguousarray(np.asarray(inputs["sgu_w"], np.float32)[:L].transpose(3, 0, 1, 2)).reshape(128, L * NH * 128)
    sgub = np.asarray(inputs["sgu_b"], np.float32)[:L].reshape(1, L * NH * 128)
    cst = _const_table(NTT)
    in_maps = []
    for c in range(8):
        b, half = c // 2, c % 2
        xc = x[b, half * NT:(half + 1) * NT, :]
        xt = np.ascontiguousarray(xc.reshape(NTT, T, 16, 128).transpose(0, 3, 2, 1)).reshape(NTT * 128, 16 * T)
        cc = cst.copy()
        cc[:, 2192] = float(half)
        in_maps.append({
            "x": xt,
            "pos": np.ascontiguousarray(pos[b, half * NT:(half + 1) * NT].astype(np.int32))[None, :],
            "wsrc": wsrc,
            "vecs": vecs,
            "sguw": sguw,
            "sgub": sgub,
            "cst": cc,
        })
    return in_maps


def assemble(results, B, NTT):
    NT = NTT * T
    out = np.empty((B, 2 * NT, D), np.float32)
    for c in range(8):
        b, half = c // 2, c % 2
        o = np.asarray(results[c]["out"]).reshape(NTT, 128, 16, T).transpose(0, 3, 2, 1).reshape(NT, D)
        out[b, half * NT:(half + 1) * NT, :] = o
    return out


def run(inputs, L, NTT, trace=False):
    nc = build_nc(L, NTT)
    in_maps = prepare(inputs, L, None, NTT)
    res = run_bass_kernel_spmd(nc, in_maps, core_ids=list(range(8)), trace=trace)
    return assemble(res.results, 4, NTT), res


def kernel(**inputs):
    out, _ = run(inputs, 4, 8)
    return out
```

```python
import math
from contextlib import ExitStack

import numpy as np
import concourse.bass as bass
import concourse.mybir as mybir
from concourse.bass_utils import run_bass_kernel_spmd

F32 = mybir.dt.float32
BF16 = mybir.dt.bfloat16
I32 = mybir.dt.int32
AF = mybir.ActivationFunctionType
ALU = mybir.AluOpType
AX = mybir.AxisListType

D = 2048
DFF = 5632
NH = 8
T = 512
NCH = T // 128
EPS = 1e-6
NBL = 240
BLK = 4096
NS = 5
DEBUG = False
WMODE = "hybrid"
VPL = 112
TWO_PI = 2.0 * math.pi
CW1 = 6.28125
CW2 = TWO_PI - CW1

O_GU1, O_DN1, O_WIN, O_BRR, O_BRS, O_OUT, O_GU2, O_DN2 = 0, 44, 76, 140, 148, 156, 164, 208


class Buf:
    __slots__ = ("name", "w", "r")

    def __init__(self, name):
        self.name = name
        self.w = None
        self.r = {}


class Sched:
    def __init__(self, nc, stack):
        self.nc = nc
        self.eng = {"pe": nc.tensor, "act": nc.scalar, "dve": nc.vector,
                    "pool": nc.gpsimd, "sp": nc.sync}
        self.sems = {}
        self.cnt = {}
        self.waited = {e: {} for e in self.eng}
        self.stack = stack
        self.dry = False
        for e in self.eng:
            self.sems["e_" + e] = stack.enter_context(nc.semaphore("sem_" + e))
            self.cnt["e_" + e] = 0
        self.n_inst = 0

    def new_sem(self, name):
        key = "d_" + name
        self.sems[key] = self.stack.enter_context(self.nc.semaphore("sem_" + name))
        self.cnt[key] = 0
        return key

    def _deps(self, reads, writes):
        deps = {}
        for b in reads:
            if b.w is not None:
                k, v = b.w
                if deps.get(k, 0) < v:
                    deps[k] = v
        for b in writes:
            if b.w is not None:
                k, v = b.w
                if deps.get(k, 0) < v:
                    deps[k] = v
            for k, v in b.r.items():
                if deps.get(k, 0) < v:
                    deps[k] = v
        return deps

    def _wait(self, e, deps, embed=True):
        own = "e_" + e
        need = []
        for k, v in deps.items():
            if k == own and (e == "pe" or v > self.cnt[own]):
                continue
            if self.waited[e].get(k, 0) >= v:
                continue
            need.append((k, v))
            self.waited[e][k] = v
        emb = need.pop() if (embed and need) else None
        for k, v in need:
            self.eng[e].wait_ge(self.sems[k], v)
        return emb

    def _mark(self, tok, reads, writes):
        for b in reads:
            if b.r.get(tok[0], 0) < tok[1]:
                b.r[tok[0]] = tok[1]
        for b in writes:
            b.w = tok
            b.r = {}

    def op(self, e, fn, reads=(), writes=(), inc=True):
        if self.dry:
            return None
        emb = self._wait(e, self._deps(reads, writes))
        ins = fn(self.eng[e])
        if emb is not None:
            ins._wait_ge(self.sems[emb[0]], emb[1])
        self.n_inst += 1
        key = "e_" + e
        if inc:
            self.cnt[key] += 1
            ins.then_inc(self.sems[key], 1)
            tok = (key, self.cnt[key])
        else:
            tok = (key, self.cnt[key] + 1)
        self._mark(tok, reads, writes)
        return ins

    def dma(self, q, out, in_, semkey, reads=(), writes=()):
        if self.dry:
            return None
        emb = self._wait(q, self._deps(reads, writes))
        ins = self.eng[q].dma_start(out=out, in_=in_)
        if emb is not None:
            ins._wait_ge(self.sems[emb[0]], emb[1])
        self.n_inst += 1
        self.cnt[semkey] += 16
        ins.then_inc(self.sems[semkey], 16)
        self._mark((semkey, self.cnt[semkey]), reads, writes)
        return ins

    def wait_all(self, e, bufs):
        if self.dry:
            return
        self._wait(e, self._deps(bufs, bufs), embed=False)


def gammas():
    h = np.arange(NH, dtype=np.float64)
    return np.log1p(-np.exp2(-5.0 - h))


def build_nc(L, NTT):
    NT = NTT * T
    NB = L * NBL
    lg = gammas()
    gamma_c = [float(np.exp(128.0 * lg[h])) for h in range(NH)]

    nc = bass.Bass("TRN2", target_bir_lowering=False)
    x_in = nc.dram_tensor("x", [NTT * 128, 16 * T], F32, kind="ExternalInput").ap()
    pos_in = nc.dram_tensor("pos", [1, NT], I32, kind="ExternalInput").ap()
    wsrc = nc.dram_tensor("wsrc", [NB * 128, BLK], F32, kind="ExternalInput").ap()
    vecs_in = nc.dram_tensor("vecs", [128, L * VPL + 16], F32, kind="ExternalInput").ap()
    sguw_in = nc.dram_tensor("sguw", [128, L * NH * 128], F32, kind="ExternalInput").ap()
    sgub_in = nc.dram_tensor("sgub", [1, L * NH * 128], F32, kind="ExternalInput").ap()
    cst_in = nc.dram_tensor("cst", [128, 2400 + NTT * 32], F32, kind="ExternalInput").ap()
    out_d = nc.dram_tensor("out", [NTT * 128, 16 * T], F32, kind="ExternalOutput").ap()
    if DEBUG:
        dbgA = nc.dram_tensor("dbgA", [NTT * 128, 16 * T], F32, kind="ExternalOutput").ap()
        dbgM = nc.dram_tensor("dbgM", [NTT * 128, 16 * T], F32, kind="ExternalOutput").ap()
    xs_t = nc.dram_tensor("xs", [NTT * 128, 16 * T], F32)
    wb_t = [nc.dram_tensor("wb%d" % l, [NBL * 128, BLK], BF16) for l in range(L if WMODE != "cast" else 0)]
    cs_t = nc.dram_tensor("cs", [2 * 128, NT], F32)
    cci_t = [nc.dram_tensor("cci%d" % l, [128, 4096], F32) for l in range(L)]
    cco_t = [nc.dram_tensor("cco%d" % l, [256, 4096], F32) for l in range(L)]
    xs = xs_t.ap()
    wbl = [w.ap() for w in wb_t]

    def wb_rows(b):
        return wbl[b // NBL][(b % NBL) * 128:(b % NBL + 1) * 128, :]
    cs = cs_t.ap()

    with ExitStack() as st:
        S = Sched(nc, st)

        def sb(name, shape, dt):
            return st.enter_context(nc.sbuf_tensor(name, shape, dt))

        X = sb("X", [128, 16 * T], F32)
        bX = Buf("X")
        HN = sb("HN", [128, 16 * T], BF16)
        bHN = Buf("HN")
        R = sb("R", [128, 32768], BF16)
        bG = [Buf("g%d" % i) for i in range(8)]
        SL = [sb("SL%d" % i, [128, BLK], BF16) for i in range(NS)]
        bSL = [Buf("SL%d" % i) for i in range(NS)]
        S32 = sb("S32", [128, NH * 512], F32)
        bS32 = [Buf("S32_%d" % h) for h in range(NH)]
        SB = sb("SB", [128, 4 * 512], BF16)
        bSB = [Buf("SB%d" % i) for i in range(4)]
        CS = sb("CS", [128, 2 * T], F32)
        bCS = Buf("CS")
        CST = sb("CST", [128, 2400 + NTT * 32], F32)
        bCST = Buf("CST")
        VEC = sb("VEC", [128, L * VPL + 16], F32)
        bVEC = Buf("VEC")
        WS = sb("WS", [128, NH * 128], BF16)
        bWS = Buf("WS")
        BS = sb("BS", [128, NH * 128], F32)
        bBS = Buf("BS")
        ONEB = sb("ONEB", [128, 128], BF16)
        ONEF = sb("ONEF", [128, 128], F32)
        IDB = sb("IDB", [128, 128], BF16)
        bONE = Buf("ONE")
        TMP = [sb("TMP%d" % i, [128, T], F32) for i in range(3)]
        bTMP = [Buf("TMP%d" % i) for i in range(3)]
        RSTD = sb("RSTD", [128, T], F32)
        bRSTD = Buf("RSTD")
        SQ = [sb("SQ%d" % i, [128, T], BF16) for i in range(2)]
        bSQ = [Buf("SQ%d" % i) for i in range(2)]
        PM = [sb("PM%d" % i, [128, T], BF16) for i in range(2)]
        bPM = [Buf("PM%d" % i) for i in range(2)]
        ST = sb("ST", [128, 64], F32)
        bST = Buf("ST")
        PS = [st.enter_context(nc.psum_tensor("PS%d" % i, [128, 512], F32)) for i in range(8)]
        bPS = [Buf("PS%d" % i) for i in range(8)]
        psi = [0]

        def nb():
            i = psi[0] % 8
            psi[0] += 1
            return PS[i], bPS[i]

        tmi = [0]

        def ntmp():
            i = tmi[0] % 3
            tmi[0] += 1
            return TMP[i], bTMP[i]

        C_MT, C_XI, C_MASK, C_INV, C_ZT, C_FLAG, C_EPS, C_WA = 0, 1024, 2048, 2176, 2184, 2192, 2193, 2400

        def rv_bf(g0, n):
            return R[:, g0 * 4096:g0 * 4096 + n]

        def rv_f32(g0, n):
            return R[:, g0 * 4096:g0 * 4096 + 2 * n].bitcast(F32)

        Hh = rv_bf(0, 44 * T)

        def bH(fc):
            return bG[(fc * T) // 4096]

        sem_w = [S.new_sem("w%d" % i) for i in range(NS)]
        sem_cst = S.new_sem("cst")
        sem_vec = S.new_sem("vec")
        sem_pos = S.new_sem("pos")
        sem_sguw = S.new_sem("sguw")
        sem_bs = S.new_sem("bs")
        sem_cci = S.new_sem("cci")
        sem_s32 = S.new_sem("s32")
        sem_x = S.new_sem("x")
        sem_csl = [S.new_sem("csl0"), S.new_sem("csl1")]
        sem_st = S.new_sem("st")
        sem_pc = [S.new_sem("pc0"), S.new_sem("pc1")]
        sem_pcs = [S.new_sem("pcs%d" % i) for i in range(NS)]
        sem_cs = S.new_sem("cs")
        sem_cc = [S.new_sem("cc%d" % l) for l in range(L)]
        sem_out = S.new_sem("out")
        bXS = [Buf("xs%d" % t) for t in range(NTT)]
        bWB = [Buf("wb%d" % b) for b in range(NB)]
        bCSD = Buf("csd")

        class WStream:
            def __init__(self):
                self.sched = []
                self.plan = None

            def make_plan(self):
                per_layer = [[] for _ in range(L)]
                cur = 0
                for ent in self.sched:
                    if ent == "rel":
                        per_layer[cur].append(ent)
                    else:
                        cur = ent // NBL
                        per_layer[cur].append(ent)
                plan = []
                for l in range(L):
                    ents = per_layer[l]
                    nrel = sum(1 for e_ in ents if e_ == "rel")
                    if WMODE == "hybrid" and l + 1 < L:
                        nxt = list(range((l + 1) * NBL, (l + 2) * NBL))
                        every = max(1, nrel // (len(nxt) + 1))
                    else:
                        nxt, every = [], 1
                    ri = 0
                    for ent in ents:
                        if ent == "rel":
                            ri += 1
                            if nxt and ri % every == 0:
                                plan.append(("conv", nxt.pop(0)))
                        else:
                            plan.append(("use", ent))
                    for b in nxt:
                        plan.append(("conv", b))
                self.plan = plan

            def reset(self):
                if self.plan is None:
                    self.make_plan()
                self.pos = 0
                self.loaded = 0
                self.released = [False] * len(self.plan)
                self.held = []
                self.seen = set()

            def _fill(self):
                while (self.loaded < len(self.plan) and self.loaded < self.pos + NS
                       and (self.loaded < NS or self.released[self.loaded - NS])):
                    j = self.loaded
                    s = j % NS
                    kind, b = self.plan[j]
                    if kind == "conv":
                        S.dma("pool", SL[s][:], wsrc[b * 128:(b + 1) * 128, :], sem_w[s], writes=[bSL[s]])
                        S.dma("sp", wb_rows(b), SL[s][:], sem_pcs[s], reads=[bSL[s]], writes=[bWB[b]])
                        self.released[j] = True
                    elif WMODE == "cast":
                        S.dma("pool", SL[s][:], wsrc[b * 128:(b + 1) * 128, :], sem_w[s], writes=[bSL[s]])
                    elif WMODE == "hybrid" and b < NBL and b not in self.seen:
                        self.seen.add(b)
                        S.dma("pool", SL[s][:], wsrc[b * 128:(b + 1) * 128, :], sem_w[s], writes=[bSL[s]])
                        S.dma("sp", wb_rows(b), SL[s][:], sem_pcs[s], reads=[bSL[s]], writes=[bWB[b]])
                    else:
                        S.dma("sp", SL[s][:], wb_rows(b), sem_w[s], reads=[bWB[b]], writes=[bSL[s]])
                    self.loaded += 1

            def pop(self, blk):
                if S.dry:
                    self.sched.append(blk)
                    return SL[0], bSL[0]
                self._fill()
                while self.plan[self.pos][0] == "conv":
                    assert self.loaded > self.pos
                    self.pos += 1
                    self._fill()
                i = self.pos
                assert self.plan[i] == ("use", blk), (i, self.plan[i], blk)
                assert self.loaded > i, "weight slot ring exhausted (too many blocks held)"
                self.held.append(i)
                self.pos += 1
                return SL[i % NS], bSL[i % NS]

            def release(self):
                if S.dry:
                    self.sched.append("rel")
                    return
                for i in self.held:
                    self.released[i] = True
                self.held = []
                self._fill()

        ws = WStream()

        def mm(out_ap, bbank, pairs, reads):
            n = len(pairs)
            for i, (l, r) in enumerate(pairs):
                S.op("pe", lambda e: e.matmul(out_ap, l, r, start=(i == 0), stop=(i == n - 1)),
                     reads=reads, writes=[bbank], inc=(i == n - 1))

        def rstd_from(ps_ap, bps, scale):
            S.op("dve", lambda e: e.tensor_scalar(RSTD[:], ps_ap, scale, EPS, ALU.mult, ALU.add),
                 reads=[bps], writes=[bRSTD])
            S.op("act", lambda e: e.activation(RSTD[:], RSTD[:], AF.Sqrt), reads=[bRSTD], writes=[bRSTD])
            S.op("dve", lambda e: e.reciprocal(RSTD[:], RSTD[:]), reads=[bRSTD], writes=[bRSTD])

        def rmsnorm(gcol, out_f32=False):
            pst, bps = nb()
            for c in range(16):
                sq, bsq = SQ[c % 2], bSQ[c % 2]
                S.op("act", lambda e: e.activation(sq[:], X[:, c * T:(c + 1) * T], AF.Square),
                     reads=[bX], writes=[bsq])
                S.op("pe", lambda e: e.matmul(pst[:], ONEB[:], sq[:], start=(c == 0), stop=(c == 15)),
                     reads=[bsq, bONE], writes=[bps], inc=True)
            rstd_from(pst[:], bps, 1.0 / D)
            for c in range(16):
                if out_f32:
                    S.op("dve", lambda e: e.scalar_tensor_tensor(
                        X[:, c * T:(c + 1) * T], X[:, c * T:(c + 1) * T], VEC[:, gcol + c:gcol + c + 1],
                        RSTD[:], ALU.mult, ALU.mult), reads=[bX, bRSTD, bVEC], writes=[bX])
                else:
                    S.op("dve", lambda e: e.scalar_tensor_tensor(
                        HN[:, c * T:(c + 1) * T], X[:, c * T:(c + 1) * T], VEC[:, gcol + c:gcol + c + 1],
                        RSTD[:], ALU.mult, ALU.mult), reads=[bX, bRSTD, bVEC], writes=[bHN])

        def ffn(base, o_gu, o_dn):
            for fb in range(22):
                Wa, bWa = ws.pop(base + o_gu + fb)
                Wg, bWg = ws.pop(base + o_gu + 22 + fb)
                for fc in range(2):
                    pa, bpa = nb()
                    pg, bpg = nb()
                    mm(pa[:], bpa, [(Wa[:, kc * 256 + fc * 128: kc * 256 + fc * 128 + 128], HN[:, kc * T:(kc + 1) * T])
                                    for kc in range(16)], [bWa, bHN])
                    mm(pg[:], bpg, [(Wg[:, kc * 256 + fc * 128: kc * 256 + fc * 128 + 128], HN[:, kc * T:(kc + 1) * T])
                                    for kc in range(16)], [bWg, bHN])
                    tm, btm = ntmp()
                    S.op("act", lambda e: e.activation(tm[:], pg[:], AF.Silu), reads=[bpg], writes=[btm])
                    f = fb * 2 + fc
                    S.op("dve", lambda e: e.tensor_tensor(Hh[:, f * T:(f + 1) * T], pa[:], tm[:], ALU.mult),
                         reads=[bpa, btm], writes=[bH(f)])
                ws.release()
            for dc in range(16):
                po, bpo = nb()
                for half in range(2):
                    Wd, bWd = ws.pop(base + o_dn + dc * 2 + half)
                    for fl in range(22):
                        f = half * 22 + fl
                        S.op("pe", lambda e: e.matmul(po[:], Wd[:, fl * 128:(fl + 1) * 128], Hh[:, f * T:(f + 1) * T],
                                                      start=(f == 0), stop=(f == 43)),
                             reads=[bWd, bH(f)], writes=[bpo], inc=(fl == 21))
                    ws.release()
                S.op("dve", lambda e: e.scalar_tensor_tensor(
                    X[:, dc * T:(dc + 1) * T], po[:], 0.5, X[:, dc * T:(dc + 1) * T], ALU.mult, ALU.add),
                    reads=[bpo, bX], writes=[bX])

        def rotary(pa, bpa, pb, bpb, dst1, dst2, bdst):
            cos = CS[:, 0:T]
            sin = CS[:, T:2 * T]
            t0, bt0 = TMP[0], bTMP[0]
            t1, bt1 = TMP[1], bTMP[1]
            S.op("dve", lambda e: e.tensor_tensor(t0[:], pa[:], cos, ALU.mult), reads=[bpa, bCS], writes=[bt0])
            S.op("dve", lambda e: e.tensor_tensor(t1[:], pb[:], sin, ALU.mult), reads=[bpb, bCS], writes=[bt1])
            S.op("dve", lambda e: e.tensor_tensor(dst1, t0[:], t1[:], ALU.subtract), reads=[bt0, bt1], writes=[bdst])
            S.op("dve", lambda e: e.tensor_tensor(t0[:], pa[:], sin, ALU.mult), reads=[bpa, bCS], writes=[bt0])
            S.op("dve", lambda e: e.tensor_tensor(t1[:], pb[:], cos, ALU.mult), reads=[bpb, bCS], writes=[bt1])
            S.op("dve", lambda e: e.tensor_tensor(dst2, t0[:], t1[:], ALU.add), reads=[bt0, bt1], writes=[bdst])

        def proj_fm(W, bW, fc):
            p, bp = nb()
            mm(p[:], bp, [(W[:, kc * 256 + fc * 128: kc * 256 + fc * 128 + 128], HN[:, kc * T:(kc + 1) * T])
                          for kc in range(16)], [bW, bHN])
            return p, bp

        def proj_head_rot(blk, dst, bdst, hl):
            W, bW = ws.pop(blk)
            pa, bpa = proj_fm(W, bW, 0)
            pb, bpb = proj_fm(W, bW, 1)
            ws.release()
            rotary(pa, bpa, pb, bpb, dst[:, (2 * hl) * T:(2 * hl + 1) * T], dst[:, (2 * hl + 1) * T:(2 * hl + 2) * T], bdst)

        def proj_v_tm(blk, dst, bdst, col0, width_total, func=None, accum_col=None):
            W, bW = ws.pop(blk)
            for ch in range(NCH):
                p, bp = nb()
                mm(p[:, 0:256], bp, [(HN[:, kc * T + ch * 128: kc * T + ch * 128 + 128], W[:, kc * 256:(kc + 1) * 256])
                                     for kc in range(16)], [bW, bHN])
                o = dst[:, ch * width_total + col0: ch * width_total + col0 + 256]
                if func is None:
                    S.op("act", lambda e: e.activation(o, p[:, 0:256], AF.Copy), reads=[bp], writes=[bdst[ch]])
                else:
                    S.op("act", lambda e: e.activation(o, p[:, 0:256], func, accum_out=ST[:, accum_col(ch):accum_col(ch) + 1]),
                         reads=[bp], writes=[bdst[ch], bST])
            ws.release()

        def program():
            if not S.dry:
                ws.reset()
            S.dma("sp", CST[:], cst_in, sem_cst, writes=[bCST])
            S.dma("sp", VEC[:], vecs_in, sem_vec, writes=[bVEC])
            S.op("dve", lambda e: e.memset(ONEF[:], 1.0), writes=[bONE])
            S.op("dve", lambda e: e.tensor_copy(ONEB[:], ONEF[:]), reads=[bONE], writes=[bONE])
            S.op("dve", lambda e: e.tensor_copy(IDB[:], CST[:, 2208:2336]), reads=[bCST, bONE], writes=[bONE])
            for t in range(NTT):
                PI_, bPI = TMP[2], bTMP[2]
                pi_i = PI_[:].bitcast(I32)
                S.dma("sp", pi_i, pos_in[:, t * T:(t + 1) * T].partition_broadcast(128), sem_pos, writes=[bPI])
                ang, bang = TMP[0], bTMP[0]
                S.op("dve", lambda e: e.tensor_copy(ang[:], pi_i), reads=[bPI], writes=[bang])
                S.op("dve", lambda e: e.tensor_scalar(ang[:], ang[:], CST[:, C_INV:C_INV + 1], None, ALU.mult),
                     reads=[bang, bCST], writes=[bang])
                for which in range(2):
                    a2, ba2 = TMP[1], bTMP[1]
                    shift = (math.pi / 2.0) if which == 0 else 0.0
                    S.op("dve", lambda e: e.tensor_scalar(a2[:], ang[:], shift, 1.0 / TWO_PI, ALU.add, ALU.mult),
                         reads=[bang], writes=[ba2])
                    S.op("dve", lambda e: e.tensor_copy(pi_i, a2[:]), reads=[ba2], writes=[bPI])
                    S.op("dve", lambda e: e.tensor_copy(a2[:], pi_i), reads=[bPI], writes=[ba2])
                    r, br = RSTD, bRSTD
                    S.op("dve", lambda e: e.scalar_tensor_tensor(r[:], a2[:], -CW1, ang[:], ALU.mult, ALU.add),
                         reads=[ba2, bang], writes=[br])
                    S.op("dve", lambda e: e.scalar_tensor_tensor(r[:], a2[:], -CW2, r[:], ALU.mult, ALU.add),
                         reads=[ba2, br], writes=[br])
                    S.op("dve", lambda e: e.tensor_scalar(r[:], r[:], shift, None, ALU.add), reads=[br], writes=[br])
                    S.op("dve", lambda e: e.tensor_scalar(r[:], r[:], math.pi, -math.pi, ALU.min, ALU.max),
                         reads=[br], writes=[br])
                    S.op("act", lambda e: e.activation(CS[:, which * T:(which + 1) * T], r[:], AF.Sin),
                         reads=[br], writes=[bCS])
                for which in range(2):
                    S.dma("sp", cs[which * 128:(which + 1) * 128, t * T:(t + 1) * T], CS[:, which * T:(which + 1) * T],
                          sem_cs, reads=[bCS], writes=[bCSD])
            STG = [X[:, 0:BLK], X[:, BLK:2 * BLK]]
            bSTG = [Buf("stg0"), Buf("stg1")]
            S.wait_all("sp", [bX])
            for b in range(NB if WMODE == "precast" else 0):
                sgi = b % 2
                sl = b % NS
                S.dma("sp", STG[sgi], wsrc[b * 128:(b + 1) * 128, :], sem_pc[sgi], writes=[bSTG[sgi], bX])
                if b % 2 == 0:
                    S.op("act", lambda e: e.activation(SL[sl][:], STG[sgi], AF.Copy), reads=[bSTG[sgi], bX], writes=[bSL[sl]])
                else:
                    S.op("dve", lambda e: e.tensor_copy(SL[sl][:], STG[sgi]), reads=[bSTG[sgi], bX], writes=[bSL[sl]])
                S.dma("sp", wb_rows(b), SL[sl][:], sem_pcs[sl], reads=[bSL[sl]], writes=[bWB[b]])

            for l in range(L):
                base = l * NBL
                vb = l * VPL
                tmw, btmw = TMP[2], bTMP[2]
                for half in range(2):
                    S.dma("sp", tmw[:], sguw_in[:, (l * NH + half * 4) * 128:(l * NH + half * 4 + 4) * 128], sem_sguw, writes=[btmw])
                    S.op("dve", lambda e: e.tensor_tensor(
                        WS[:, half * 512:(half + 1) * 512].rearrange("p (g i) -> p g i", g=4),
                        tmw[:].rearrange("p (g i) -> p g i", g=4),
                        CST[:, C_MASK:C_MASK + 128].unsqueeze(1).to_broadcast([128, 4, 128]), ALU.mult),
                        reads=[btmw, bCST], writes=[bWS])
                S.dma("sp", BS[:], sgub_in[:, l * NH * 128:(l + 1) * NH * 128].partition_broadcast(128), sem_bs, writes=[bBS])
                for h in range(NH):
                    S.op("dve", lambda e: e.memset(S32[:, h * 512:(h + 1) * 512], 0.0), writes=[bS32[h]])

                KT = rv_bf(0, 16 * T)
                bKT = Buf("KTa")
                KW = rv_bf(2, 4 * 2048)
                VV = rv_bf(4, 4 * 2048)
                for t in range(NTT):
                    src = x_in if l == 0 else xs
                    S.dma("sp", X[:], src[t * 128:(t + 1) * 128, :], sem_x, reads=[bXS[t]], writes=[bX])
                    for which in range(2):
                        S.dma("sp", CS[:, which * T:(which + 1) * T], cs[which * 128:(which + 1) * 128, t * T:(t + 1) * T],
                              sem_csl[which], reads=[bCSD], writes=[bCS])
                    rmsnorm(vb + 0)
                    ffn(base, O_GU1, O_DN1)
                    S.dma("sp", xs[t * 128:(t + 1) * 128, :], X[:], sem_st, reads=[bX], writes=[bXS[t]])
                    if DEBUG and l == 0:
                        S.dma("sp", dbgA[t * 128:(t + 1) * 128, :], X[:], sem_out, reads=[bX])
                    rmsnorm(vb + 16)
                    bk = [bG[0], bG[1]]
                    for h in range(NH):
                        W, bW = ws.pop(base + O_WIN + 8 + h)
                        pa, bpa = proj_fm(W, bW, 0)
                        pb, bpb = proj_fm(W, bW, 1)
                        ws.release()
                        rotary(pa, bpa, pb, bpb, KT[:, (2 * h) * T:(2 * h + 1) * T], KT[:, (2 * h + 1) * T:(2 * h + 2) * T], bk[h // 4])
                    for h in range(NH):
                        proj_v_tm(base + O_WIN + 16 + h, VV, [bG[4], bG[4], bG[5], bG[5]], h * 256, 2048)
                    bKTall = Buf("x")

                    def tabA(ch, h0, hcnt, t=t):
                        n = t * NCH + ch
                        return CST[:, C_WA + n * 8 + h0: C_WA + n * 8 + h0 + hcnt].unsqueeze(2).to_broadcast([128, hcnt, 256])
                    for ch in range(NCH):
                        for half in range(2):
                            p, bp = nb()
                            pbv = p[:].bitcast(BF16)
                            for c8 in range(8):
                                c = half * 8 + c8
                                S.op("pe", lambda e: e.transpose(pbv[:, c8 * 128:(c8 + 1) * 128],
                                                                 KT[:, c * T + ch * 128: c * T + ch * 128 + 128], IDB[:]),
                                     reads=[bk[half], bONE], writes=[bp], inc=(c8 == 7))
                            o = KW[:, ch * 2048 + half * 1024: ch * 2048 + half * 1024 + 1024]
                            S.op("dve", lambda e: e.tensor_tensor(
                                o.rearrange("p (h d) -> p h d", h=4),
                                pbv[:, 0:1024].rearrange("p (h d) -> p h d", h=4),
                                tabA(ch, half * 4, 4), ALU.mult), reads=[bp, bCST], writes=[bG[2 + ch // 2]])
                    for h in range(NH):
                        pu, bpu = nb()
                        for dc in range(2):
                            mm(pu[:, dc * 256:(dc + 1) * 256], bpu,
                               [(KW[:, ch * 2048 + h * 256 + dc * 128: ch * 2048 + h * 256 + dc * 128 + 128],
                                 VV[:, ch * 2048 + h * 256: ch * 2048 + (h + 1) * 256]) for ch in range(NCH)],
                               [bG[2], bG[3], bG[4], bG[5]])
                        S.op("dve", lambda e: e.tensor_tensor(S32[:, h * 512:(h + 1) * 512], S32[:, h * 512:(h + 1) * 512],
                                                              pu[:], ALU.add), reads=[bpu, bS32[h]], writes=[bS32[h]])

                bcci = Buf("cci")
                bcco = Buf("cco")
                S.dma("pool", cci_t[l].ap(), S32[:], sem_cci, reads=bS32, writes=[bcci])
                S.wait_all("pool", [bcci])
                if not S.dry:
                    ins = nc.gpsimd.collective_compute("AllGather", ALU.bypass,
                                                       replica_groups=[[0, 1], [2, 3], [4, 5], [6, 7]],
                                                       ins=[cci_t[l].ap().opt()], outs=[cco_t[l].ap().opt()])
                    S.cnt[sem_cc[l]] += 1
                    ins.then_inc(S.sems[sem_cc[l]], 1)
                    bcco.w = (sem_cc[l], 1)
                S.dma("sp", S32[:], cco_t[l].ap()[0:128, :], sem_s32, reads=[bcco], writes=bS32)
                for h in range(NH):
                    S.op("dve", lambda e: e.tensor_scalar(S32[:, h * 512:(h + 1) * 512], S32[:, h * 512:(h + 1) * 512],
                                                          CST[:, C_FLAG:C_FLAG + 1], None, ALU.mult),
                         reads=[bS32[h], bCST], writes=[bS32[h]])

                QT = rv_bf(0, 8 * T)
                KT2 = rv_bf(1, 8 * T)
                KZ = rv_bf(2, 4 * 1024)
                V4 = rv_bf(3, 4 * 1024)
                YP = rv_f32(4, 8 * T)
                YR = rv_bf(6, 16 * T)
                VG = rv_f32(0, 4 * 2048)
                VLN = rv_bf(4, 4 * 2048)
                YS = rv_bf(0, 16 * T)
                MG = rv_bf(2, 16 * T)
                last = (l == L - 1)
                for t in range(NTT):
                    S.dma("sp", X[:], xs[t * 128:(t + 1) * 128, :], sem_x, reads=[bXS[t]], writes=[bX])
                    for which in range(2):
                        S.dma("sp", CS[:, which * T:(which + 1) * T], cs[which * 128:(which + 1) * 128, t * T:(t + 1) * T],
                              sem_csl[which], reads=[bCSD], writes=[bCS])
                    rmsnorm(vb + 16)
                    for hh in range(2):
                        for hl in range(4):
                            h = hh * 4 + hl
                            proj_head_rot(base + O_WIN + h, QT, bG[0], hl)
                        for hl in range(4):
                            h = hh * 4 + hl
                            proj_head_rot(base + O_WIN + 8 + h, KT2, bG[1], hl)
                        for hl in range(4):
                            h = hh * 4 + hl
                            proj_v_tm(base + O_WIN + 16 + h, V4, [bG[3]] * 4, hl * 256, 1024)
                        for ch in range(NCH):
                            p, bp = nb()
                            pbv = p[:].bitcast(BF16)
                            for c in range(8):
                                S.op("pe", lambda e: e.transpose(pbv[:, c * 128:(c + 1) * 128],
                                                                 KT2[:, c * T + ch * 128: c * T + ch * 128 + 128], IDB[:]),
                                     reads=[bG[1], bONE], writes=[bp], inc=(c == 7))
                            S.op("dve", lambda e: e.tensor_tensor(
                                KZ[:, ch * 1024:(ch + 1) * 1024].rearrange("p (h d) -> p h d", h=4),
                                pbv[:, 0:1024].rearrange("p (h d) -> p h d", h=4),
                                CST[:, C_ZT + hh * 4: C_ZT + hh * 4 + 4].unsqueeze(2).to_broadcast([128, 4, 256]), ALU.mult),
                                reads=[bp, bCST], writes=[bG[2]])
                        for hl in range(4):
                            h = hh * 4 + hl
                            U = []
                            for ch in range(NCH):
                                pu, bpu = nb()
                                for dc in range(2):
                                    mm(pu[:, dc * 256:(dc + 1) * 256], bpu,
                                       [(KZ[:, ch * 1024 + hl * 256 + dc * 128: ch * 1024 + hl * 256 + dc * 128 + 128],
                                         V4[:, ch * 1024 + hl * 256: ch * 1024 + (hl + 1) * 256])], [bG[2], bG[3]])
                                U.append((pu, bpu))
                            psc, bpsc = nb()
                            for ch in range(NCH):
                                mm(psc[:, ch * 128:(ch + 1) * 128], bpsc,
                                   [(KT2[:, (2 * hl + dc) * T + ch * 128: (2 * hl + dc) * T + ch * 128 + 128],
                                     QT[:, (2 * hl + dc) * T + ch * 128: (2 * hl + dc) * T + ch * 128 + 128]) for dc in range(2)],
                                   [bG[0], bG[1]])
                            pm, bpm = PM[h % 2], bPM[h % 2]
                            S.op("dve", lambda e: e.tensor_tensor(
                                pm[:].rearrange("p (c i) -> p c i", c=4), psc[:].rearrange("p (c i) -> p c i", c=4),
                                CST[:, C_MT + h * 128: C_MT + (h + 1) * 128].unsqueeze(1).to_broadcast([128, 4, 128]), ALU.mult),
                                reads=[bpsc, bCST], writes=[bpm])
                            for ch in range(NCH):
                                S.op("act", lambda e: e.activation(SB[:, ch * 512:(ch + 1) * 512], S32[:, h * 512:(h + 1) * 512], AF.Copy),
                                     reads=[bS32[h]], writes=[bSB[ch]])
                                pu, bpu = U[ch]
                                S.op("dve", lambda e: e.scalar_tensor_tensor(
                                    S32[:, h * 512:(h + 1) * 512], S32[:, h * 512:(h + 1) * 512], gamma_c[h], pu[:], ALU.mult, ALU.add),
                                    reads=[bS32[h], bpu], writes=[bS32[h]])
                            for ec in range(2):
                                py, bpy = nb()
                                for ch in range(NCH):
                                    prs = [(V4[:, ch * 1024 + hl * 256 + ec * 128: ch * 1024 + hl * 256 + ec * 128 + 128],
                                            pm[:, ch * 128:(ch + 1) * 128])]
                                    for dc in range(2):
                                        prs.append((SB[:, ch * 512 + dc * 256 + ec * 128: ch * 512 + dc * 256 + ec * 128 + 128],
                                                    QT[:, (2 * hl + dc) * T + ch * 128: (2 * hl + dc) * T + ch * 128 + 128]))
                                    mm(py[:, ch * 128:(ch + 1) * 128], bpy, prs, [bG[3], bpm, bSB[ch], bG[0]])
                                c = 2 * hl + ec
                                S.op("dve", lambda e: e.tensor_tensor(
                                    YP[:, c * T:(c + 1) * T].rearrange("p (c i) -> p c i", c=4), py[:].rearrange("p (c i) -> p c i", c=4),
                                    CST[:, C_XI + h * 128: C_XI + (h + 1) * 128].unsqueeze(1).to_broadcast([128, 4, 128]), ALU.mult),
                                    reads=[bpy, bCST], writes=[bG[4 + c // 4]])
                            p1, bp1 = nb()
                            p2, bp2 = nb()
                            sqs = []
                            for ec in range(2):
                                c = 2 * hl + ec
                                tq, btq = TMP[ec], bTMP[ec]
                                S.op("act", lambda e: e.activation(tq[:], YP[:, c * T:(c + 1) * T], AF.Square),
                                     reads=[bG[4 + c // 4]], writes=[btq])
                                sqs.append((tq, btq))
                            for ec in range(2):
                                c = 2 * hl + ec
                                S.op("pe", lambda e: e.matmul(p1[:], ONEF[:], YP[:, c * T:(c + 1) * T], start=(ec == 0), stop=(ec == 1)),
                                     reads=[bG[4 + c // 4], bONE], writes=[bp1], inc=(ec == 1))
                            for ec in range(2):
                                tq, btq = sqs[ec]
                                S.op("pe", lambda e: e.matmul(p2[:], ONEF[:], tq[:], start=(ec == 0), stop=(ec == 1)),
                                     reads=[btq, bONE], writes=[bp2], inc=(ec == 1))
                            mean, bmean = TMP[2], bTMP[2]
                            S.op("dve", lambda e: e.tensor_scalar(mean[:], p1[:], 1.0 / 256.0, None, ALU.mult), reads=[bp1], writes=[bmean])
                            m2, bm2 = TMP[0], bTMP[0]
                            S.op("act", lambda e: e.activation(m2[:], p1[:], AF.Square, scale=1.0 / 256.0), reads=[bp1], writes=[bm2])
                            S.op("dve", lambda e: e.scalar_tensor_tensor(RSTD[:], p2[:], 1.0 / 256.0, m2[:], ALU.mult, ALU.subtract),
                                 reads=[bp2, bm2], writes=[bRSTD])
                            S.op("dve", lambda e: e.tensor_scalar(RSTD[:], RSTD[:], 0.0, None, ALU.max), reads=[bRSTD], writes=[bRSTD])
                            S.op("act", lambda e: e.activation(RSTD[:], RSTD[:], AF.Sqrt, bias=CST[:, C_EPS:C_EPS + 1]),
                                 reads=[bRSTD, bCST], writes=[bRSTD])
                            S.op("dve", lambda e: e.reciprocal(RSTD[:], RSTD[:]), reads=[bRSTD], writes=[bRSTD])
                            for ec in range(2):
                                c = 2 * hl + ec
                                S.op("dve", lambda e: e.tensor_tensor(YP[:, c * T:(c + 1) * T], YP[:, c * T:(c + 1) * T], mean[:], ALU.subtract),
                                     reads=[bG[4 + c // 4], bmean], writes=[bG[4 + c // 4]])
                                S.op("dve", lambda e: e.tensor_tensor(YP[:, c * T:(c + 1) * T], YP[:, c * T:(c + 1) * T], RSTD[:], ALU.mult),
                                     reads=[bG[4 + c // 4], bRSTD], writes=[bG[4 + c // 4]])
                        for hl in range(4):
                            h = hh * 4 + hl
                            W, bW = ws.pop(base + O_WIN + 24 + h)
                            for ec in range(2):
                                pg, bpg = proj_fm(W, bW, ec)
                                tm, btm = ntmp()
                                S.op("act", lambda e: e.activation(tm[:], pg[:], AF.Silu), reads=[bpg], writes=[btm])
                                c = 2 * hl + ec
                                cg = 2 * h + ec
                                S.op("dve", lambda e: e.scalar_tensor_tensor(
                                    YR[:, cg * T:(cg + 1) * T], YP[:, c * T:(c + 1) * T], VEC[:, vb + 64 + cg: vb + 64 + cg + 1],
                                    tm[:], ALU.mult, ALU.mult), reads=[bG[4 + c // 4], btm, bVEC], writes=[bG[6 + cg // 8]])
                            ws.release()
                    bVG = [bG[0], bG[1], bG[2], bG[3]]
                    S.op("dve", lambda e: e.memset(ST[:], 0.0), writes=[bST])
                    for blk in range(8):
                        proj_v_tm(base + O_WIN + 40 + blk, VG, bVG, blk * 256, 2048, func=AF.Gelu,
                                  accum_col=lambda ch, blk=blk: ch * 8 + blk)
                    for ch in range(NCH):
                        S.op("dve", lambda e: e.tensor_reduce(ST[:, 32 + ch:33 + ch], ST[:, ch * 8:(ch + 1) * 8], AX.X, ALU.add),
                             reads=[bST], writes=[bST])
                        S.op("dve", lambda e: e.tensor_scalar(ST[:, 32 + ch:33 + ch], ST[:, 32 + ch:33 + ch], -1.0 / D, None, ALU.mult),
                             reads=[bST], writes=[bST])
                        S.op("act", lambda e: e.activation(VLN[:, ch * 2048:(ch + 1) * 2048], VG[:, ch * 2048:(ch + 1) * 2048], AF.Square,
                                                           bias=ST[:, 32 + ch:33 + ch], accum_out=ST[:, 36 + ch:37 + ch]),
                             reads=[bVG[ch], bST], writes=[bG[4 + ch // 2], bST])
                        S.op("dve", lambda e: e.tensor_scalar(ST[:, 40 + ch:41 + ch], ST[:, 36 + ch:37 + ch], 1.0 / D, EPS, ALU.mult, ALU.add),
                             reads=[bST], writes=[bST])
                        S.op("act", lambda e: e.activation(ST[:, 40 + ch:41 + ch], ST[:, 40 + ch:41 + ch], AF.Sqrt), reads=[bST], writes=[bST])
                        S.op("dve", lambda e: e.reciprocal(ST[:, 40 + ch:41 + ch], ST[:, 40 + ch:41 + ch]), reads=[bST], writes=[bST])
                        S.op("dve", lambda e: e.tensor_scalar(VLN[:, ch * 2048:(ch + 1) * 2048], VG[:, ch * 2048:(ch + 1) * 2048],
                                                              ST[:, 32 + ch:33 + ch], ST[:, 40 + ch:41 + ch], ALU.add, ALU.mult),
                             reads=[bVG[ch], bST], writes=[bG[4 + ch // 2]])
                    for blk in range(8):
                        W, bW = ws.pop(base + O_WIN + 32 + blk)
                        g = blk
                        for ec in range(2):
                            c = blk * 2 + ec
                            pmx, bpmx = nb()
                            for ch in range(NCH):
                                mm(pmx[:, ch * 128:(ch + 1) * 128], bpmx,
                                   [(VLN[:, ch * 2048 + c * 128: ch * 2048 + (c + 1) * 128], WS[:, g * 128:(g + 1) * 128])],
                                   [bG[4 + ch // 2], bWS])
                            pu, bpu = proj_fm(W, bW, ec)
                            tm, btm = ntmp()
                            S.op("act", lambda e: e.activation(tm[:], pu[:], AF.Gelu), reads=[bpu], writes=[btm])
                            t2, bt2 = ntmp()
                            S.op("dve", lambda e: e.scalar_tensor_tensor(
                                t2[:].rearrange("p (c i) -> p c i", c=4), pmx[:].rearrange("p (c i) -> p c i", c=4),
                                VEC[:, vb + 80 + c: vb + 80 + c + 1],
                                BS[:, g * 128:(g + 1) * 128].unsqueeze(1).to_broadcast([128, 4, 128]), ALU.mult, ALU.add),
                                reads=[bpmx, bVEC, bBS], writes=[bt2])
                            S.op("dve", lambda e: e.tensor_tensor(YS[:, c * T:(c + 1) * T], t2[:], tm[:], ALU.mult),
                                 reads=[bt2, btm], writes=[bG[c // 8]])
                        ws.release()
                    for blk in range(8):
                        Wr, bWr = ws.pop(base + O_BRR + blk)
                        Wsg, bWsg = ws.pop(base + O_BRS + blk)
                        Wgr, bWgr = ws.pop(base + O_WIN + 48 + blk)
                        Wgs, bWgs = ws.pop(base + O_WIN + 56 + blk)
                        for ec in range(2):
                            c = blk * 2 + ec
                            pr, bpr = nb()
                            mm(pr[:], bpr, [(Wr[:, kc * 256 + ec * 128: kc * 256 + ec * 128 + 128], YR[:, kc * T:(kc + 1) * T])
                                            for kc in range(16)], [bWr, bG[6], bG[7]])
                            pss, bpss = nb()
                            mm(pss[:], bpss, [(Wsg[:, kc * 256 + ec * 128: kc * 256 + ec * 128 + 128], YS[:, kc * T:(kc + 1) * T])
                                              for kc in range(16)], [bWsg, bG[0], bG[1]])
                            pgr, bpgr = proj_fm(Wgr, bWgr, ec)
                            pgs, bpgs = proj_fm(Wgs, bWgs, ec)
                            t0, bt0 = TMP[0], bTMP[0]
                            t1, bt1 = TMP[1], bTMP[1]
                            S.op("act", lambda e: e.activation(t0[:], pgr[:], AF.Sigmoid, bias=VEC[:, vb + 32 + c: vb + 32 + c + 1]),
                                 reads=[bpgr, bVEC], writes=[bt0])
                            S.op("act", lambda e: e.activation(t1[:], pgs[:], AF.Sigmoid, bias=VEC[:, vb + 48 + c: vb + 48 + c + 1]),
                                 reads=[bpgs, bVEC], writes=[bt1])
                            S.op("dve", lambda e: e.tensor_tensor(t0[:], t0[:], pr[:], ALU.mult), reads=[bt0, bpr], writes=[bt0])
                            S.op("dve", lambda e: e.tensor_tensor(t1[:], t1[:], pss[:], ALU.mult), reads=[bt1, bpss], writes=[bt1])
                            S.op("dve", lambda e: e.tensor_tensor(MG[:, c * T:(c + 1) * T], t0[:], t1[:], ALU.add),
                                 reads=[bt0, bt1], writes=[bG[2 + c // 8]])
                        ws.release()
                    for blk in range(8):
                        W, bW = ws.pop(base + O_OUT + blk)
                        for ec in range(2):
                            c = blk * 2 + ec
                            po, bpo = nb()
                            mm(po[:], bpo, [(W[:, kc * 256 + ec * 128: kc * 256 + ec * 128 + 128], MG[:, kc * T:(kc + 1) * T])
                                            for kc in range(16)], [bW, bG[2], bG[3]])
                            S.op("dve", lambda e: e.tensor_tensor(X[:, c * T:(c + 1) * T], X[:, c * T:(c + 1) * T], po[:], ALU.add),
                                 reads=[bpo, bX], writes=[bX])
                        ws.release()
                    if DEBUG and l == 0:
                        S.dma("sp", dbgM[t * 128:(t + 1) * 128, :], X[:], sem_out, reads=[bX])
                    rmsnorm(vb + 96)
                    ffn(base, O_GU2, O_DN2)
                    if last:
                        rmsnorm(L * VPL, out_f32=True)
                        S.dma("sp", out_d[t * 128:(t + 1) * 128, :], X[:], sem_out, reads=[bX])
                    else:
                        S.dma("sp", xs[t * 128:(t + 1) * 128, :], X[:], sem_st, reads=[bX], writes=[bXS[t]])
            if not S.dry:
                S.eng["sp"].wait_ge(S.sems[sem_out], S.cnt[sem_out])

        S.dry = True
        program()
        S.dry = False
        psi[0] = 0
        tmi[0] = 0
        program()
        build_nc.last_n_inst = S.n_inst
    return nc


def _blocks_2048(W):
    F = W.shape[1]
    return W.reshape(16, 128, F // 256, 256).transpose(2, 1, 0, 3).reshape(F // 256, 128, 4096)


def _blocks_down(W):
    a = W.reshape(2, 22, 128, 16, 128).transpose(3, 0, 2, 1, 4).reshape(32, 128, 22 * 128)
    out = np.zeros((32, 128, 4096), np.float32)
    out[:, :, :22 * 128] = a
    return out


def _const_table(NTT):
    NT = NTT * T
    lg = gammas()
    cst = np.zeros((128, 2400 + NTT * 32), np.float64)
    j = np.arange(128)[:, None]
    i = np.arange(128)[None, :]
    for h in range(NH):
        cst[:, h * 128:(h + 1) * 128] = np.where(i >= j, np.exp(-(j + 1.0) * lg[h]), 0.0) / 16.0
        cst[:, 1024 + h * 128: 1024 + (h + 1) * 128] = np.exp((i + 1.0) * lg[h])
        cst[:, 2184 + h] = np.exp((127.0 - np.arange(128)) * lg[h]) / 16.0
    cst[:, 2048:2176] = (j <= i)
    cst[:, 2176] = 10000.0 ** (-np.arange(128) / 128.0)
    cst[:, 2193] = EPS
    cst[:, 2208:2336] = np.eye(128)
    for n in range(NT // 128):
        for h in range(NH):
            cst[:, 2400 + n * 8 + h] = np.exp((NT - 1.0 - (n * 128 + np.arange(128))) * lg[h]) / 16.0
    return cst.astype(np.float32)


def prepare(inputs, L, S_total, NTT):
    NT = NTT * T
    x = np.asarray(inputs["x"])
    pos = np.asarray(inputs["positions"])
    blocks = []
    for l in range(L):
        blocks.append(_blocks_2048(np.asarray(inputs["ffn1_w_gu"][l])))
        blocks.append(_blocks_down(np.asarray(inputs["ffn1_w_down"][l])))
        blocks.append(_blocks_2048(np.asarray(inputs["w_in"][l])))
        blocks.append(_blocks_2048(np.asarray(inputs["w_branch_ret"][l])))
        blocks.append(_blocks_2048(np.asarray(inputs["w_branch_sgu"][l])))
        blocks.append(_blocks_2048(np.asarray(inputs["w_out"][l])))
        blocks.append(_blocks_2048(np.asarray(inputs["ffn2_w_gu"][l])))
        blocks.append(_blocks_down(np.asarray(inputs["ffn2_w_down"][l])))
    wsrc = np.concatenate(blocks, axis=0).reshape(L * NBL * 128, BLK)
    del blocks

    def pc(v):
        return np.asarray(v, np.float32).reshape(16, 128).T

    vecs = np.zeros((128, L * VPL + 16), np.float32)
    for l in range(L):
        b = l * VPL
        vecs[:, b + 0:b + 16] = pc(inputs["ffn1_norm"][l])
        vecs[:, b + 16:b + 32] = pc(inputs["mix_norm"][l])
        vecs[:, b + 32:b + 48] = pc(np.asarray(inputs["b_gate"][l])[:D])
        vecs[:, b + 48:b + 64] = pc(np.asarray(inputs["b_gate"][l])[D:])
        vecs[:, b + 64:b + 80] = pc(inputs["ret_gn"][l])
        vecs[:, b + 80:b + 96] = pc(inputs["sgu_ln"][l])
        vecs[:, b + 96:b + 112] = pc(inputs["ffn2_norm"][l])
    vecs[:, L * VPL:] = pc(inputs["final_norm"])
    sguw = np.ascontiguousarray(np.asarray(inputs["sgu_w"], np.float32)[:L].transpose(3, 0, 1, 2)).reshape(128, L * NH * 128)
    sgub = np.asarray(inputs["sgu_b"], np.float32)[:L].reshape(1, L * NH * 128)
    cst = _const_table(NTT)
    in_maps = []
    for c in range(8):
        b, half = c // 2, c % 2
        xc = x[b, half * NT:(half + 1) * NT, :]
        xt = np.ascontiguousarray(xc.reshape(NTT, T, 16, 128).transpose(0, 3, 2, 1)).reshape(NTT * 128, 16 * T)
        cc = cst.copy()
        cc[:, 2192] = float(half)
        in_maps.append({
            "x": xt,
            "pos": np.ascontiguousarray(pos[b, half * NT:(half + 1) * NT].astype(np.int32))[None, :],
            "wsrc": wsrc,
            "vecs": vecs,
            "sguw": sguw,
            "sgub": sgub,
            "cst": cc,
        })
    return in_maps


def assemble(results, B, NTT):
    NT = NTT * T
    out = np.empty((B, 2 * NT, D), np.float32)
    for c in range(8):
        b, half = c // 2, c % 2
        o = np.asarray(results[c]["out"]).reshape(NTT, 128, 16, T).transpose(0, 3, 2, 1).reshape(NT, D)
        out[b, half * NT:(half + 1) * NT, :] = o
    return out


def run(inputs, L, NTT, trace=False):
    nc = build_nc(L, NTT)
    in_maps = prepare(inputs, L, None, NTT)
    res = run_bass_kernel_spmd(nc, in_maps, core_ids=list(range(8)), trace=trace)
    return assemble(res.results, 4, NTT), res


def kernel(**inputs):
    out, _ = run(inputs, 4, 8)
    return out
```
